# Optimizing a Trainium2 kernel written in Bass

```python
import jax, jax.numpy as jnp
from jax import lax
import numpy as np

D_MODEL = 1024
BATCH = 8
SEQ = 4096
DEPTH = 2

GLA_HEADS = 4
GLA_DK = 32
GLA_DV = 64
GLA_RANK = 16
GLA_GATE_NORM = 16.0
GLA_CHUNK = 64
RET_HEADS = 4
RET_DK = 64
RET_DV = 64
RET_CHUNK = 128
ROPE_BASE = 10000.0
SB_HEADS = 8
SB_DK = 64
SB_DV = 64
SB_BLOCK = 128
D_MIX = GLA_HEADS * GLA_DV + RET_HEADS * RET_DV + SB_HEADS * SB_DV
D_FF = -(-8 * D_MODEL // (3 * 256)) * 256
ADA_SCALE = 0.5
EPS = 1e-6

IN_SIZES = (GLA_HEADS * GLA_DK, GLA_HEADS * GLA_DK, GLA_HEADS * GLA_DV, GLA_HEADS * GLA_DV, GLA_RANK,
            RET_HEADS * RET_DK, RET_HEADS * RET_DK, RET_HEADS * RET_DV, RET_HEADS * RET_DV,
            SB_HEADS * SB_DK, SB_HEADS * SB_DK, SB_HEADS * SB_DV)
IN_COLS = sum(IN_SIZES)
SPLIT_POINTS = tuple(int(s) for s in np.cumsum(IN_SIZES)[:-1])

kernel_name = "hybrid_gla_retnet_stickbreak_adaln"


def rms_norm(x, g):
    xf = x.astype(jnp.float32)
    y = xf * lax.rsqrt(jnp.mean(xf * xf, axis=-1, keepdims=True) + EPS)
    return (y * g.astype(jnp.float32)).astype(x.dtype)


def gla_mixer(q, k, v, log_a):
    B, S, H, dk = q.shape
    dv = v.shape[-1]
    C = GLA_CHUNK
    N = S // C

    def to_chunks(t):
        return t.reshape(B, N, C, H, t.shape[-1]).transpose(1, 0, 3, 2, 4)

    qc, kc, vc, ac = (to_chunks(t) for t in (q * dk ** -0.5, k, v, log_a))
    causal = jnp.tril(jnp.ones((C, C), dtype=bool))[:, :, None]

    def step(state, inp):
        qi, ki, vi, ai = inp
        b = jnp.cumsum(ai, axis=2)
        diff = b[:, :, :, None, :] - b[:, :, None, :, :]
        decay = jnp.exp(jnp.where(causal, diff, -jnp.inf))
        scores = jnp.einsum('bhtd,bhsd,bhtsd->bhts', qi, ki, decay)
        o = (jnp.einsum('bhts,bhsv->bhtv', scores, vi)
             + jnp.einsum('bhtd,bhdv->bhtv', qi * jnp.exp(b), state))
        b_last = b[:, :, -1:, :]
        state = (jnp.exp(b_last[:, :, 0, :, None]) * state
                 + jnp.einsum('bhsd,bhsv->bhdv', ki * jnp.exp(b_last - b), vi))
        return state, o

    state0 = jnp.zeros((B, H, dk, dv), jnp.float32)
    _, o = lax.scan(step, state0, (qc, kc, vc, ac))
    return o.transpose(1, 0, 3, 2, 4).reshape(B, S, H, dv)


def rotary(t, pos):
    d = t.shape[-1]
    inv = ROPE_BASE ** (-jnp.arange(0, d, 2, dtype=jnp.float32) / d)
    ang = pos.astype(jnp.float32)[:, None] * inv[None, :]
    cos = jnp.cos(ang)[None, :, None, :]
    sin = jnp.sin(ang)[None, :, None, :]
    t1, t2 = t[..., 0::2], t[..., 1::2]
    return jnp.stack([t1 * cos - t2 * sin, t1 * sin + t2 * cos], axis=-1).reshape(t.shape)


def retention_mixer(q, k, v):
    B, S, H, dk = q.shape
    dv = v.shape[-1]
    C = RET_CHUNK
    N = S // C
    pos = jnp.arange(S)
    q = rotary(q, pos)
    k = rotary(k, pos) * dk ** -0.5
    log_g = jnp.log(1.0 - 2.0 ** (-5.0 - jnp.arange(H, dtype=jnp.float32)))

    def to_chunks(t):
        return t.reshape(B, N, C, H, t.shape[-1]).transpose(0, 3, 1, 2, 4)

    qc, kc, vc = to_chunks(q), to_chunks(k), to_chunks(v)
    idx = jnp.arange(C, dtype=jnp.float32)
    rel = idx[:, None] - idx[None, :]
    dmat = jnp.where(rel >= 0, jnp.exp(log_g[:, None, None] * jnp.maximum(rel, 0.0)), 0.0)
    scores = jnp.einsum('bhntd,bhnsd->bhnts', qc, kc) * dmat[None, :, None]
    o_intra = jnp.einsum('bhnts,bhnsv->bhntv', scores, vc)

    zeta = jnp.exp(log_g[:, None] * (C - 1.0 - idx)[None, :])
    kv = jnp.einsum('bhnsd,bhnsv->nbhdv', kc * zeta[None, :, None, :, None], vc)
    gamma_c = jnp.exp(log_g * C)[None, :, None, None]

    def step(r, kv_i):
        return gamma_c * r + kv_i, r

    _, r_prev = lax.scan(step, jnp.zeros((B, H, dk, dv), jnp.float32), kv)
    xi = jnp.exp(log_g[:, None] * (idx + 1.0)[None, :])
    o_inter = jnp.einsum('bhntd,nbhdv->bhntv', qc * xi[None, :, None, :, None], r_prev)
    o = o_intra + o_inter
    return o.transpose(0, 2, 3, 1, 4).reshape(B, S, H, dv)


def stick_breaking_mixer(q, k, v):
    B, S, H, d = q.shape
    T = SB_BLOCK
    q = q.transpose(0, 2, 1, 3) * d ** -0.5
    k = k.transpose(0, 2, 1, 3)
    v = v.transpose(0, 2, 1, 3)
    outs = []
    for i in range(S // T):
        end = (i + 1) * T
        qb, kb, vb = q[:, :, i * T:end], k[:, :, :end], v[:, :, :end]
        z = jnp.einsum('bhtd,bhsd->bhts', qb, kb)
        mask = jnp.arange(end)[None, :] < (i * T + jnp.arange(T))[:, None]
        log_1m = jnp.where(mask, jax.nn.log_sigmoid(-z), 0.0)
        log_a = jax.nn.log_sigmoid(z) + lax.cumsum(log_1m, axis=3, reverse=True) - log_1m
        attn = jnp.where(mask, jnp.exp(log_a), 0.0)
        outs.append(jnp.einsum('bhts,bhsv->bhtv', attn, vb))
    o = jnp.concatenate(outs, axis=2)
    return o.transpose(0, 2, 1, 3)


def hybrid_mixer(h, w_in, gla_wa2, gla_ba, gla_norm_g, ret_norm_g, w_out):
    B, S, _ = h.shape
    proj = jnp.einsum('bsd,de->bse', h, w_in).astype(jnp.float32)
    gq, gk, gv, gg, gr, rq, rk, rv, rg, sq, sk, sv = jnp.split(proj, SPLIT_POINTS, axis=-1)

    def heads(t, n):
        return t.reshape(B, S, n, -1)

    log_a = jax.nn.log_sigmoid(gr @ gla_wa2.astype(jnp.float32) + gla_ba.astype(jnp.float32)) / GLA_GATE_NORM
    o_gla = gla_mixer(heads(gq, GLA_HEADS), heads(gk, GLA_HEADS), heads(gv, GLA_HEADS), heads(log_a, GLA_HEADS))
    o_gla = rms_norm(o_gla, gla_norm_g).reshape(B, S, -1) * jax.nn.silu(gg)
    o_ret = retention_mixer(heads(rq, RET_HEADS), heads(rk, RET_HEADS), heads(rv, RET_HEADS))
    o_ret = rms_norm(o_ret, ret_norm_g).reshape(B, S, -1) * jax.nn.silu(rg)
    o_sb = stick_breaking_mixer(heads(sq, SB_HEADS), heads(sk, SB_HEADS), heads(sv, SB_HEADS)).reshape(B, S, -1)

    o = jnp.concatenate([o_gla, o_ret, o_sb], axis=-1).astype(h.dtype)
    return jnp.einsum('bse,ed->bsd', o, w_out)


def swiglu(h, wg, wu, wd):
    a = jnp.einsum('bsd,df->bsf', h, wg)
    b = jnp.einsum('bsd,df->bsf', h, wu)
    return jnp.einsum('bsf,fd->bsd', jax.nn.silu(a) * b, wd)


def setup_inputs(seed: int = 0) -> dict:
    key = jax.random.key(seed)
    ks = jax.random.split(key, 16)
    L, D = DEPTH, D_MODEL

    def nrm(k, shape, fan_in):
        return jax.random.normal(k, shape, jnp.float32) * fan_in ** -0.5

    def small(k, shape):
        return 0.02 * jax.random.normal(k, shape, jnp.float32)

    return {
        'x': jax.random.normal(ks[0], (BATCH, SEQ, D), jnp.float32),
        'c': jax.random.normal(ks[1], (BATCH, D), jnp.float32),
        'ada_w': nrm(ks[2], (L, D, 6 * D), D) * ADA_SCALE,
        'ada_b': small(ks[3], (L, 6 * D)),
        'norm1_g': 1.0 + small(ks[4], (L, D)),
        'norm2_g': 1.0 + small(ks[5], (L, D)),
        'w_in': nrm(ks[6], (L, D, IN_COLS), D),
        'gla_wa2': nrm(ks[7], (L, GLA_RANK, GLA_HEADS * GLA_DK), GLA_RANK),
        'gla_ba': small(ks[8], (L, GLA_HEADS * GLA_DK)),
        'gla_norm_g': 1.0 + small(ks[9], (L, GLA_DV)),
        'ret_norm_g': 1.0 + small(ks[10], (L, RET_DV)),
        'w_out': nrm(ks[11], (L, D_MIX, D), D_MIX),
        'ffn_wg': nrm(ks[12], (L, D, D_FF), D),
        'ffn_wu': nrm(ks[13], (L, D, D_FF), D),
        'ffn_wd': nrm(ks[14], (L, D_FF, D), D_FF),
        'final_g': 1.0 + small(ks[15], (D,)),
    }


def reference(x, c, ada_w, ada_b, norm1_g, norm2_g, w_in, gla_wa2, gla_ba, gla_norm_g,
              ret_norm_g, w_out, ffn_wg, ffn_wu, ffn_wd, final_g):
    cs = jax.nn.silu(c)
    for l in range(DEPTH):
        mod = cs @ ada_w[l] + ada_b[l]
        sh1, sc1, g1, sh2, sc2, g2 = (m[:, None, :] for m in jnp.split(mod, 6, axis=-1))
        h = rms_norm(x, norm1_g[l]) * (1.0 + sc1) + sh1
        x = x + g1 * hybrid_mixer(h, w_in[l], gla_wa2[l], gla_ba[l], gla_norm_g[l], ret_norm_g[l], w_out[l])
        h = rms_norm(x, norm2_g[l]) * (1.0 + sc2) + sh2
        x = x + g2 * swiglu(h, ffn_wg[l], ffn_wu[l], ffn_wd[l])
    return rms_norm(x, final_g)
```

```python
import contextlib
import numpy as np
import ml_dtypes
import concourse.bass as bass
import concourse.mybir as mybir
from concourse.bass_utils import run_bass_kernel_spmd

F32 = mybir.dt.float32
BF16 = mybir.dt.bfloat16
U8 = mybir.dt.uint8
AF = mybir.ActivationFunctionType
ALU = mybir.AluOpType
AX = mybir.AxisListType
_ISZ = {F32: 4, BF16: 2, U8: 1}

D = 1024
DEPTH = 2
DFF = 2816
NF = DFF // 128
INC = 3344
EPS = 1e-6
NEG = -30000.0


class _Op:
    __slots__ = ("eng", "fn", "deps", "sig", "sigval", "dsem", "dval", "isdma", "gi")

    def __init__(self, eng, fn, isdma, gi):
        self.eng = eng
        self.fn = fn
        self.deps = set()
        self.sig = False
        self.sigval = 0
        self.dsem = -1
        self.dval = 0
        self.isdma = isdma
        self.gi = gi


class Prog:
    ENGS = ("pe", "act", "dve", "pool", "sp")
    NDMA = 40

    def __init__(self, nc, ro_names=()):
        self.nc = nc
        self.ops = []
        self.track = {}
        self.ro = set(ro_names)
        self.dma_rr = 0
        self.dma_last = [None] * self.NDMA
        self.dma_cnt = [0] * self.NDMA
        self.last_on = {e: None for e in self.ENGS}

    def _box(self, ap):
        t = ap.tensor
        ps = 1
        for s in list(t.shape)[1:]:
            ps *= int(s)
        isz = _ISZ[ap.dtype]
        off = int(ap.offset)
        p0 = off // ps
        f0 = off % ps
        pe = 0
        fe = 0
        for (st, cnt) in ap.ap:
            st = int(st)
            cnt = int(cnt)
            if cnt <= 1 or st == 0:
                continue
            if st % ps == 0:
                pe += (cnt - 1) * (st // ps)
            else:
                fe += (cnt - 1) * st
        if t.name == "PS":
            lo = (f0 * isz) // 2048 * 2048
            hi = -(-((f0 + fe + 1) * isz) // 2048) * 2048
            return (t.name, 0, 128, lo, hi)
        return (t.name, p0, p0 + pe + 1, f0 * isz, (f0 + fe + 1) * isz)

    def _access(self, op, ap, is_write):
        name, p0, p1, b0, b1 = self._box(ap)
        if name in self.ro:
            return
        d = self.track.setdefault(name, {})
        ekey = ("dma", op.gi) if op.isdma else op.eng
        dead = []
        for key, oi in d.items():
            (w, ek, q0, q1, c0, c1) = key
            if not (w or is_write):
                continue
            if q1 <= p0 or p1 <= q0 or c1 <= b0 or b1 <= c0:
                continue
            if oi == op.gi:
                continue
            if ek == ekey and op.eng == "pe":
                pass
            else:
                op.deps.add(oi)
            if is_write and q0 >= p0 and q1 <= p1 and c0 >= b0 and c1 <= b1:
                dead.append(key)
        for k in dead:
            del d[k]
        d[(is_write, ekey, p0, p1, b0, b1)] = op.gi

    def add(self, eng, fn, reads=(), writes=(), isdma=False):
        op = _Op(eng, fn, isdma, len(self.ops))
        self.ops.append(op)
        for ap in reads:
            if ap is not None and not isinstance(ap, (int, float)):
                self._access(op, ap, False)
        for ap in writes:
            self._access(op, ap, True)
        if isdma:
            k = self.dma_rr
            self.dma_rr = (self.dma_rr + 1) % self.NDMA
            if self.dma_last[k] is not None:
                op.deps.add(self.dma_last[k])
            self.dma_last[k] = op.gi
            self.dma_cnt[k] += 16
            op.dsem = k
            op.dval = self.dma_cnt[k]
            op.sig = True
        op.deps.discard(op.gi)
        for dgi in op.deps:
            self.ops[dgi].sig = True
        self.last_on[eng] = op.gi
        return op

    def barrier(self):
        lasts = [gi for gi in self.last_on.values() if gi is not None]
        dmas = [gi for gi in self.dma_last if gi is not None]
        for e in self.ENGS:
            op = _Op(e, None, False, len(self.ops))
            self.ops.append(op)
            for gi in lasts + dmas:
                o = self.ops[gi]
                if o.fn is None or (o.eng == e and not o.isdma):
                    continue
                op.deps.add(gi)
                o.sig = True

    def mm(self, out, lhsT, rhs, start=True, stop=True, **kw):
        return self.add("pe", lambda e: e.matmul(out, lhsT, rhs, start=start, stop=stop, **kw),
                        reads=[lhsT, rhs], writes=[out])

    def transpose(self, out, in_, ident):
        return self.add("pe", lambda e: e.transpose(out, in_, ident), reads=[in_, ident], writes=[out])

    def act(self, out, in_, func, bias=None, scale=None, accum_out=None):
        kw = {}
        rd = [in_]
        wr = [out]
        if bias is not None:
            kw["bias"] = bias
            rd.append(bias)
        if scale is not None:
            kw["scale"] = scale
            rd.append(scale)
        if accum_out is not None:
            kw["accum_out"] = accum_out
            wr.append(accum_out)
        return self.add("act", lambda e: e.activation(out, in_, func, **kw), reads=rd, writes=wr)

    def tt(self, out, in0, in1, op, eng="dve"):
        return self.add(eng, lambda e: e.tensor_tensor(out, in0, in1, op), reads=[in0, in1], writes=[out])

    def ts(self, out, in0, s1, s2, op0, op1=None, eng="dve"):
        kw = {}
        if op1 is not None:
            kw["op1"] = op1
        return self.add(eng, lambda e: e.tensor_scalar(out, in0, s1, s2, op0, **kw),
                        reads=[in0, s1, s2], writes=[out])

    def stt(self, out, in0, scalar, in1, op0, op1):
        return self.add("dve", lambda e: e.scalar_tensor_tensor(out, in0, scalar, in1, op0, op1),
                        reads=[in0, scalar, in1], writes=[out])

    def copy(self, out, in_, eng="dve"):
        if eng == "act":
            return self.add("act", lambda e: e.copy(out, in_), reads=[in_], writes=[out])
        return self.add(eng, lambda e: e.tensor_copy(out, in_), reads=[in_], writes=[out])

    def memset(self, ap, val, eng="dve"):
        return self.add(eng, lambda e: e.memset(ap, val), writes=[ap])

    def reduce(self, out, in_, axis, op):
        return self.add("dve", lambda e: e.tensor_reduce(out, in_, axis, op), reads=[in_], writes=[out])

    def recip(self, out, in_):
        return self.add("dve", lambda e: e.reciprocal(out, in_), reads=[in_], writes=[out])

    def dma(self, out, in_, eng="sp", **kw):
        return self.add(eng, lambda e: e.dma_start(out=out, in_=in_, **kw), reads=[in_], writes=[out],
                        isdma=True)

    def emit(self):
        nc = self.nc
        cnt = {e: 0 for e in self.ENGS}
        for op in self.ops:
            if op.isdma or op.fn is None:
                continue
            if op.sig:
                cnt[op.eng] += 1
                op.sigval = cnt[op.eng]
        with contextlib.ExitStack() as st:
            esem = {e: st.enter_context(nc.semaphore("es_" + e)) for e in self.ENGS}
            dsem = [st.enter_context(nc.semaphore("ds%d" % i)) for i in range(self.NDMA)]
            block = st.enter_context(nc.Block())
            handles = {"pe": block.tensor, "act": block.scalar, "dve": block.vector,
                       "pool": block.gpsimd, "sp": block.sync}
            ops = self.ops

            def make(ename):
                def body(eng):
                    seen = {}
                    for op in ops:
                        if op.eng != ename:
                            continue
                        need = {}
                        for dgi in op.deps:
                            d = ops[dgi]
                            if d.isdma:
                                key = ("d", d.dsem)
                                val = d.dval
                            else:
                                if d.fn is None:
                                    continue
                                key = ("e", d.eng)
                                val = d.sigval
                            if val > need.get(key, 0):
                                need[key] = val
                        for key, val in need.items():
                            if seen.get(key, 0) >= val:
                                continue
                            seen[key] = val
                            sem = dsem[key[1]] if key[0] == "d" else esem[key[1]]
                            eng.wait_ge(sem, val)
                        if op.fn is None:
                            continue
                        inst = op.fn(eng)
                        if op.isdma:
                            inst.then_inc(dsem[op.dsem], 16)
                        elif op.sig:
                            inst.then_inc(esem[op.eng], 1)
                return body

            for ename in self.ENGS:
                handles[ename](make(ename))


class Arena:
    def __init__(self, ap_u8, size):
        self.t = ap_u8
        self.size = size
        self.top = 0
        self.peak = 0

    def mark(self):
        return self.top

    def release(self, m):
        self.top = m

    def alloc(self, dtype, shape, parts=None):
        n = 1
        for s in shape[1:]:
            n *= s
        nb = n * _ISZ[dtype]
        off = (self.top + 63) // 64 * 64
        assert off + nb <= self.size, ("arena overflow", off, nb, self.size)
        self.top = off + nb
        self.peak = max(self.peak, self.top)
        v = self.t[0:shape[0], off:off + nb].bitcast(dtype)
        if len(shape) == 3:
            v = v.rearrange("p (a b) -> p a b", b=shape[2])
        elif len(shape) == 4:
            v = v.rearrange("p (a b c) -> p a b c", b=shape[2], c=shape[3])
        return v


def _consts(S):
    NB = S // 128
    c = {}
    idx = np.arange(128)
    c["ident"] = np.eye(128, dtype=np.float32)
    c["m_incl"] = (idx[:, None] <= idx[None, :]).astype(np.float32)
    c["negstrict"] = np.where(idx[:, None] >= idx[None, :], NEG, 0.0).astype(np.float32)
    c["ntri"] = -(idx[:, None] >= idx[None, :]).astype(np.float32)
    c["nones"] = -np.ones((128, 128), np.float32)
    c["trig"] = c["m_incl"] * (-1.0 / 16.0)
    c["n16"] = np.full((128, 1), -1.0 / 16.0, np.float32)
    inv = 10000.0 ** (-np.arange(0, 64, 2, dtype=np.float32) / 64.0)
    pos = np.arange(S, dtype=np.float32)
    ang = pos[:, None] * inv[None, :]
    c["cos"] = np.cos(ang).astype(np.float32).reshape(NB, 128, 32).transpose(1, 0, 2).copy()
    c["sin"] = np.sin(ang).astype(np.float32).reshape(NB, 128, 32).transpose(1, 0, 2).copy()
    gam = 1.0 - 2.0 ** (-5.0 - np.arange(4, dtype=np.float64))
    tl = np.arange(128, dtype=np.float64)
    qs = gam[None, :] ** (tl[:, None] + 1.0)
    ks = gam[None, :] ** (-(tl[:, None] + 1.0)) / 8.0
    c["rsc"] = np.concatenate([qs, ks], axis=1).astype(np.float32)
    gc = gam ** 128.0
    c["retD"] = np.stack([np.repeat(gc[0:2], 64), np.repeat(gc[2:4], 64)], axis=1).astype(np.float32)
    bm = np.zeros((128, 256), np.float32)
    for h in range(4):
        bm[32 * h:32 * h + 32, 64 * h:64 * h + 64] = 1.0
    c["bm_gla"] = bm
    bm2 = np.zeros((128, 128), np.float32)
    for h in range(2):
        bm2[64 * h:64 * h + 64, 64 * h:64 * h + 64] = 1.0
    c["bm_ret"] = bm2
    c["onerow"] = np.ones((1, 128), np.float32)
    return c


_CONST_SHAPES = None


def build(S, dbg=False, nlayers=DEPTH, phases=(0, 1, 2, 3)):
    NB = S // 128
    NQC = S // 512
    nc = bass.Bass("TRN2", target_bir_lowering=False)
    cst = _consts(S)
    ins = {}

    def din(name, shape, dt=F32):
        ins[name] = nc.dram_tensor(name, list(shape), dt, kind="ExternalInput").ap()
        return ins[name]

    x_d = din("x", [S, D])
    cT_d = din("cT", [128, 8])
    adaw_d = din("ada_w", [DEPTH, D, 6 * D])
    adab_d = din("ada_b", [DEPTH, 1, 6 * D])
    n1g_d = din("norm1_g", [DEPTH, 1, D])
    n2g_d = din("norm2_g", [DEPTH, 1, D])
    win_d = din("w_in", [DEPTH, D, INC])
    wa2_d = din("gla_wa2", [DEPTH, 16, 128])
    gba_d = din("gla_ba", [DEPTH, 1, 128])
    glag_d = din("gla_norm_g", [DEPTH, 1, 64])
    retg_d = din("ret_norm_g", [DEPTH, 1, 64])
    wout_d = din("w_out", [DEPTH, D, D])
    wg_d = din("ffn_wg", [DEPTH, D, DFF])
    wu_d = din("ffn_wu", [DEPTH, D, DFF])
    wd_d = din("ffn_wd", [DEPTH, DFF, D])
    fg_d = din("final_g", [1, D])
    cd = {k: din("c_" + k, v.shape) for k, v in cst.items()}
    out_d = nc.dram_tensor("out", [S, D], F32, kind="ExternalOutput").ap()
    xs_d = nc.dram_tensor("xs", [S, D], F32, kind="Internal").ap()
    hT_d = nc.dram_tensor("hT", [D, S], BF16, kind="Internal").ap()
    oT_d = nc.dram_tensor("oT", [D, S], BF16, kind="Internal").ap()
    mod_d = nc.dram_tensor("modrow", [DEPTH, 1, 6 * D], F32, kind="Internal").ap()
    wd2_d = nc.dram_tensor("wd2", [DEPTH, DFF, D], BF16, kind="Internal").ap()
    dbg_d = {}
    if dbg:
        dbg_d["d_hT"] = nc.dram_tensor("d_hT", [D, S], BF16, kind="ExternalOutput").ap()
        dbg_d["d_oT"] = nc.dram_tensor("d_oT", [D, S], BF16, kind="ExternalOutput").ap()
        dbg_d["d_mod"] = nc.dram_tensor("d_mod", [DEPTH, 6, D], F32, kind="ExternalOutput").ap()
        dbg_d["d_x1"] = nc.dram_tensor("d_x1", [S, D], F32, kind="ExternalOutput").ap()

    P = Prog(nc, ro_names=list(ins.keys()))
    SBYTES = 204 * 1024
    with contextlib.ExitStack() as st:
        SBt = st.enter_context(nc.sbuf_tensor("SB", [128, SBYTES], U8))
        PSt = st.enter_context(nc.psum_tensor("PS", [128, 16384], U8))
        A = Arena(SBt, SBYTES)

        def psb(b, dt=F32):
            return PSt[:, b * 2048:(b + 1) * 2048].bitcast(dt)

        ident_bf = A.alloc(BF16, [128, 128])
        ident_f = A.alloc(F32, [128, 128])
        m_incl = A.alloc(F32, [128, 128])
        negstrict = A.alloc(BF16, [128, 128])
        ntri = A.alloc(BF16, [128, 128])
        nones = A.alloc(BF16, [128, 128])
        trig = A.alloc(F32, [128, 128])
        n16 = A.alloc(F32, [128, 1])
        cos_t = A.alloc(F32, [128, NB, 32])
        sin_t = A.alloc(F32, [128, NB, 32])
        rsc = A.alloc(F32, [128, 8])
        retD = A.alloc(F32, [128, 2])
        bm_gla = A.alloc(F32, [128, 256])
        bm_ret = A.alloc(F32, [128, 128])
        onerow = A.alloc(F32, [1, 128])
        one11 = onerow[0:1, 0:1]
        mc = A.mark()
        for dst, nm in ((ident_bf, "ident"), (negstrict, "negstrict"), (ntri, "ntri"), (nones, "nones")):
            cs_ = A.alloc(F32, [128, 128])
            P.dma(cs_, cd[nm])
            P.copy(dst, cs_)
        A.release(mc)
        P.dma(ident_f, cd["ident"])
        P.dma(m_incl, cd["m_incl"])
        P.dma(trig, cd["trig"])
        P.dma(n16, cd["n16"])
        P.dma(cos_t, cd["cos"])
        P.dma(sin_t, cd["sin"])
        P.dma(rsc, cd["rsc"])
        P.dma(retD, cd["retD"])
        P.dma(bm_gla, cd["bm_gla"])
        P.dma(bm_ret, cd["bm_ret"])
        P.dma(onerow, cd["onerow"])
        epsc = A.alloc(F32, [128, 1])
        P.memset(epsc, EPS)
        warm_rhs = A.alloc(BF16, [128, 512])
        P.memset(warm_rhs, 0.0)

        m0 = A.mark()
        cT = A.alloc(F32, [128, 8])
        csT = A.alloc(F32, [128, 8])
        tmp8 = A.alloc(F32, [128, 8])
        row = A.alloc(F32, [1, 6 * D])
        grow = A.alloc(F32, [1, 2 * D + 128])
        brow = A.alloc(F32, [1, 6 * D])
        wbuf = [A.alloc(F32, [128, 8, 512]) for _ in range(2)]
        P.dma(cT, cT_d)
        P.act(tmp8, cT, AF.Exp, scale=-1.0)
        P.ts(tmp8, tmp8, 1.0, None, ALU.add)
        P.recip(tmp8, tmp8)
        P.tt(csT, cT, tmp8, ALU.mult)
        for l in range(nlayers):
            P.dma(brow, adab_d[l])
            for cg in range(12):
                wb = wbuf[cg % 2]
                P.dma(wb, adaw_d[l][:, cg * 512:(cg + 1) * 512].rearrange("(k p) n -> p k n", p=128),
                      eng="sp")
                ps = psb(cg % 2)
                for k in range(8):
                    P.mm(ps[0:1, :], csT[:, k:k + 1], wb[:, k, :], start=(k == 0), stop=(k == 7))
                P.tt(row[0:1, cg * 512:(cg + 1) * 512], ps[0:1, :], brow[0:1, cg * 512:(cg + 1) * 512], ALU.add)
            P.dma(grow[0:1, 0:D], n1g_d[l])
            P.dma(grow[0:1, D:2 * D], n2g_d[l])
            P.stt(row[0:1, D:2 * D], row[0:1, D:2 * D], 1.0, grow[0:1, 0:D], ALU.add, ALU.mult)
            P.stt(row[0:1, 4 * D:5 * D], row[0:1, 4 * D:5 * D], 1.0, grow[0:1, D:2 * D], ALU.add, ALU.mult)
            if dbg:
                P.dma(dbg_d["d_mod"][l:l + 1].rearrange("o s d -> o (s d)"), row)
            P.dma(mod_d[l], row)
        A.release(m0)

        def norm_block(xb, ab, shb, scr, hTb, psT):
            P.act(scr["junk"], xb, AF.Square, accum_out=scr["ss"])
            P.act(scr["ln"], scr["ss"], AF.Ln, scale=1.0 / D, bias=scr["eps"])
            P.act(scr["rstd"], scr["ln"], AF.Exp, scale=-0.5)
            P.stt(scr["xm"], xb, scr["rstd"], ab, ALU.mult, ALU.mult)
            P.tt(scr["xn"], scr["xm"], shb, ALU.add)
            for k in range(8):
                P.transpose(psT[:, k * 128:(k + 1) * 128], scr["xn"][:, k * 128:(k + 1) * 128], ident_bf)
            P.copy(hTb, psT.rearrange("p (a b) -> p a b", a=8), eng="act")

        def modrow(l, j):
            return mod_d[l][0:1, j * D:(j + 1) * D].to_broadcast([128, D])

        for l in range(nlayers):
            xsrc = x_d if l == 0 else xs_d
            last_layer = (l == nlayers - 1)
            ml = A.mark()
            wgs = A.alloc(BF16, [128, 8, DFF])
            cast_ctr = [0]

            def load_cast(dst, src, stg, fold=None):
                sb_ = stg[cast_ctr[0] % len(stg)]
                cast_ctr[0] += 1
                n = dst.shape[-1]
                P.dma(sb_[:, 0:n], src)
                if fold is not None:
                    P.tt(dst, sb_[:, 0:n], fold, ALU.mult)
                elif cast_ctr[0] % 2 == 0:
                    P.copy(dst, sb_[:, 0:n], eng="act")
                else:
                    P.copy(dst, sb_[:, 0:n])
            if 1 in phases:
                m1 = A.mark()
                a1b = A.alloc(F32, [128, D])
                sh1b = A.alloc(F32, [128, D])
                gnb_l = A.alloc(F32, [128, 512])
                wa2_l = A.alloc(F32, [16, 128])
                gba_l = A.alloc(F32, [1, 128])
                P.dma(a1b, modrow(l, 1))
                P.dma(sh1b, modrow(l, 0))
                for r in range(8):
                    srcg = glag_d[l] if r < 4 else retg_d[l]
                    P.dma(gnb_l[:, r * 64:(r + 1) * 64], srcg.to_broadcast([128, 64]))
                P.dma(wa2_l, wa2_d[l])
                P.dma(gba_l, gba_d[l])
                wla = A.alloc(BF16, [128, 8, 1808])
                stg1 = [A.alloc(F32, [128, 1408]) for _ in range(2)]
                for k in range(8):
                    for c0 in (0, 904):
                        load_cast(wla[:, k, c0:c0 + 904], win_d[l][k * 128:(k + 1) * 128, c0:c0 + 904], stg1)
                xbs = [A.alloc(F32, [128, D]) for _ in range(2)]
                scr = {"junk": A.alloc(BF16, [128, D]), "ss": A.alloc(F32, [128, 1]),
                       "ln": A.alloc(F32, [128, 1]), "rstd": A.alloc(F32, [128, 1]),
                       "xm": A.alloc(F32, [128, D]), "xn": A.alloc(BF16, [128, D]), "eps": epsc}
                hTb = A.alloc(BF16, [128, 8, 128])
                grT = A.alloc(F32, [16, 128])
                e1 = A.alloc(F32, [128, 128])
                lsp = A.alloc(F32, [128, 128])
                eb = A.alloc(F32, [128, 128])
                enb = A.alloc(F32, [128, 128])
                Dg = A.alloc(F32, [128, 1])
                qg = A.alloc(BF16, [128, 128])
                kg = A.alloc(BF16, [128, 128])
                KX = A.alloc(BF16, [128, 640])
                KXr = A.alloc(BF16, [128, 4, 128])
                rt = [A.alloc(F32, [128, 8, 32]) for _ in range(4)]
                qkr32 = A.alloc(F32, [128, 8, 32, 2])
                QKr = A.alloc(BF16, [128, 512])
                qkT = A.alloc(BF16, [128, 11, 128])
                scT = A.alloc(BF16, [128, 8, 128])
                Vall = A.alloc(BF16, [128, 512])
                S32 = [A.alloc(F32, [128, 256]), A.alloc(F32, [128, 128]), A.alloc(F32, [128, 128])]
                tS = [A.alloc(F32, [128, 256]), A.alloc(F32, [128, 128]), A.alloc(F32, [128, 128])]
                Sbf = [A.alloc(BF16, [128, 256]), A.alloc(BF16, [128, 128]), A.alloc(BF16, [128, 128])]
                eg = A.alloc(F32, [128, 512])
                sg = A.alloc(F32, [128, 512])
                osq = A.alloc(F32, [128, 512])
                ssum = A.alloc(F32, [128, 8])
                orstd = A.alloc(F32, [128, 8])
                on = A.alloc(F32, [128, 512])
                og = A.alloc(BF16, [128, 512])
                oTb = A.alloc(BF16, [128, 4, 128])
                P.memset(KX, 0.0)
                P.memset(KXr, 0.0)
                for i in range(3):
                    P.memset(S32[i], 0.0)
                    P.memset(Sbf[i], 0.0)
                KXd = KX.rearrange("p (h c) -> p h c", c=160)[:, :, 0:32]
                b0, b1, b2, b3, b4 = psb(0), psb(1), psb(2), psb(3), psb(4)
                b5, b6, b7 = psb(5, BF16), psb(6, BF16), psb(7, BF16)
                wg_pieces = [(k, c0) for k in range(8) for c0 in range(0, DFF, 1408)]
                for tb in range(NB):
                    xb = xbs[tb % 2]
                    P.dma(xb, xsrc[tb * 128:(tb + 1) * 128, :])
                    norm_block(xb, a1b, sh1b, scr, hTb, b5)
                    npp = -(-len(wg_pieces) // NB)
                    for (k_, c0_) in wg_pieces[tb * npp:(tb + 1) * npp]:
                        load_cast(wgs[:, k_, c0_:c0_ + 1408], wg_d[l][k_ * 128:(k_ + 1) * 128, c0_:c0_ + 1408], stg1)
                    P.dma(hT_d[:, tb * 128:(tb + 1) * 128].rearrange("(k p) t -> p k t", p=128), hTb)
                    for k in range(8):
                        P.mm(b0, hTb[:, k, :], wla[:, k, 0:512], start=(k == 0), stop=(k == 7))
                    for k in range(8):
                        P.mm(b1[:, 0:256], hTb[:, k, :], wla[:, k, 512:768], start=(k == 0), stop=(k == 7))
                    for k in range(8):
                        P.mm(b1[:, 256:384], wla[:, k, 768:896], hTb[:, k, :], start=(k == 0), stop=(k == 7))
                    for k in range(8):
                        P.mm(b2, hTb[:, k, :], wla[:, k, 784:1296], start=(k == 0), stop=(k == 7))
                    for k in range(8):
                        P.mm(b3, hTb[:, k, :], wla[:, k, 1296:1808], start=(k == 0), stop=(k == 7))
                    P.copy(grT, b1[0:16, 256:384])
                    P.copy(Vall[:, 0:256], b0[:, 256:512], eng="act")
                    P.copy(Vall[:, 256:512], b3[:, 0:256], eng="act")
                    P.act(eg[:, 0:256], b1[:, 0:256], AF.Exp, scale=-1.0)
                    P.act(eg[:, 256:512], b3[:, 256:512], AF.Exp, scale=-1.0)
                    P.act(eg, eg, AF.Ln, bias=1.0)
                    P.act(eg, eg, AF.Exp, scale=-1.0)
                    P.tt(sg[:, 0:256], b1[:, 0:256], eg[:, 0:256], ALU.mult)
                    P.tt(sg[:, 256:512], b3[:, 256:512], eg[:, 256:512], ALU.mult)
                    P.mm(b4[:, 0:128], grT, wa2_l, start=True, stop=False)
                    P.mm(b4[:, 0:128], onerow[0:1, :], gba_l, start=False, stop=True)
                    P.act(e1, b4[:, 0:128], AF.Exp, scale=-1.0)
                    P.act(lsp, e1, AF.Ln, bias=1.0)
                    P.mm(b4[:, 128:256], trig, lsp, start=True, stop=True)
                    P.mm(b4[:, 256:257], lsp, n16, start=True, stop=True)
                    P.act(eb, b4[:, 128:256], AF.Exp)
                    P.act(enb, b4[:, 128:256], AF.Exp, scale=-1.0)
                    P.act(Dg, b4[:, 256:257], AF.Exp)
                    P.stt(qg, b0[:, 0:128], 32.0 ** -0.5, eb, ALU.mult, ALU.mult)
                    P.tt(kg, b0[:, 128:256], enb, ALU.mult)
                    P.copy(KXd, kg.rearrange("p (h d) -> p h d", h=4), eng="act")
                    qk4 = b2.rearrange("p (h i two) -> p h i two", h=8, two=2)
                    ev, od = qk4[:, :, :, 0], qk4[:, :, :, 1]
                    cb = cos_t[:, tb, :].unsqueeze(1).broadcast_to([128, 8, 32])
                    sb_ = sin_t[:, tb, :].unsqueeze(1).broadcast_to([128, 8, 32])
                    P.tt(rt[0], ev, cb, ALU.mult)
                    P.tt(rt[1], od, sb_, ALU.mult)
                    P.tt(rt[2], ev, sb_, ALU.mult)
                    P.tt(rt[3], od, cb, ALU.mult)
                    P.tt(qkr32[:, :, :, 0], rt[0], rt[1], ALU.subtract)
                    P.tt(qkr32[:, :, :, 1], rt[2], rt[3], ALU.add)
                    P.tt(QKr.rearrange("p (h d) -> p h d", h=8), qkr32.rearrange("p h i two -> p h (i two)"),
                         rsc.unsqueeze(2).broadcast_to([128, 8, 64]), ALU.mult)
                    P.transpose(b6[:, 0:128], qg, ident_bf)
                    for h in range(4):
                        P.transpose(b6[:, (1 + h) * 128:(2 + h) * 128], KX[:, h * 128:(h + 1) * 128], ident_bf)
                    for h in range(4):
                        c0 = (h % 2) * 64
                        P.copy(KXr[:, h, c0:c0 + 64], QKr[:, 256 + h * 64:320 + h * 64], eng=("act" if h % 2 else "dve"))
                    for j in range(2):
                        P.transpose(b6[:, (5 + j) * 128:(6 + j) * 128], QKr[:, j * 128:(j + 1) * 128], ident_bf)
                    P.transpose(b6[:, 7 * 128:8 * 128], KXr[:, 0, :], ident_bf)
                    for h in range(1, 4):
                        P.transpose(b7[:, (h - 1) * 128:h * 128], KXr[:, h, :], ident_bf)
                    P.copy(qkT[:, 0:8, :], b6.rearrange("p (a b) -> p a b", a=8), eng="act")
                    P.copy(qkT[:, 8:11, :], b7[:, 0:384].rearrange("p (a b) -> p a b", a=3), eng="act")
                    for h in range(4):
                        P.mm(b0[:, h * 128:(h + 1) * 128], qkT[:, 1 + h, :], qkT[:, 0, :], start=True, stop=True)
                    for h in range(4):
                        P.mm(b2[:, h * 128:(h + 1) * 128], qkT[:, 7 + h, :], qkT[:, 5 + h // 2, :],
                             start=True, stop=True)
                    mb = m_incl.unsqueeze(1).broadcast_to([128, 4, 128])
                    P.tt(scT[:, 0:4, :], b0.rearrange("p (h t) -> p h t", h=4), mb, ALU.mult)
                    P.tt(scT[:, 4:8, :], b2.rearrange("p (h t) -> p h t", h=4), mb, ALU.mult)
                    for h in range(8):
                        if h < 4:
                            ql, sl = qkT[:, 0, :], Sbf[0][:, h * 64:(h + 1) * 64]
                        else:
                            g = (h - 4) // 2
                            ql, sl = qkT[:, 5 + g, :], Sbf[1 + g][:, ((h - 4) % 2) * 64:((h - 4) % 2 + 1) * 64]
                        P.mm(b1[:, h * 64:(h + 1) * 64], scT[:, h, :], Vall[:, h * 64:(h + 1) * 64],
                             start=True, stop=False)
                        P.mm(b1[:, h * 64:(h + 1) * 64], ql, sl, start=False, stop=True)
                    P.mm(b3[:, 0:256], kg, Vall[:, 0:256], start=True, stop=True)
                    P.mm(b3[:, 256:384], QKr[:, 256:384], Vall[:, 256:384], start=True, stop=True)
                    P.mm(b3[:, 384:512], QKr[:, 384:512], Vall[:, 384:512], start=True, stop=True)
                    P.tt(tS[0], b3[:, 0:256], S32[0], ALU.add)
                    P.stt(S32[0], tS[0], Dg, bm_gla, ALU.mult, ALU.mult)
                    P.copy(Sbf[0], S32[0], eng="act")
                    for g in range(2):
                        P.tt(tS[1 + g], b3[:, 256 + g * 128:384 + g * 128], S32[1 + g], ALU.add)
                        P.stt(S32[1 + g], tS[1 + g], retD[:, g:g + 1], bm_ret, ALU.mult, ALU.mult)
                        P.copy(Sbf[1 + g], S32[1 + g], eng="act")
                    P.act(osq, b1, AF.Square)
                    P.reduce(ssum, osq.rearrange("p (h v) -> p h v", h=8), AX.X, ALU.add)
                    P.act(orstd, ssum, AF.Ln, scale=1.0 / 64.0, bias=epsc)
                    P.act(orstd, orstd, AF.Exp, scale=-0.5)
                    P.tt(on.rearrange("p (h v) -> p h v", h=8), b1.rearrange("p (h v) -> p h v", h=8),
                         orstd.unsqueeze(2).broadcast_to([128, 8, 64]), ALU.mult)
                    P.tt(on, on, gnb_l, ALU.mult)
                    P.tt(og, on, sg, ALU.mult)
                    for j in range(4):
                        P.transpose(b7[:, (3 + j) * 128:(4 + j) * 128], og[:, j * 128:(j + 1) * 128], ident_bf)
                    P.copy(oTb, b7[:, 384:896].rearrange("p (a b) -> p a b", a=4), eng="act")
                    P.dma(oT_d[0:512, tb * 128:(tb + 1) * 128].rearrange("(k p) t -> p k t", p=128), oTb)
                A.release(m1)

            wus = A.alloc(BF16, [128, 8, DFF])
            wo = A.alloc(BF16, [128, 8, D])
            if 2 in phases:
                m2 = A.mark()
                wsb = A.alloc(BF16, [128, 8, 384])
                hTc = [A.alloc(BF16, [128, 8, 512]) for _ in range(1)]
                QT = A.alloc(BF16, [128, S])
                KTz = [A.alloc(BF16, [128, S]) for _ in range(2)]
                P.memset(KTz[0][64:128, :], 0.0)
                P.memset(KTz[1][0:64, :], 0.0)
                Vp = A.alloc(BF16, [128, NB, 128])
                ez = [A.alloc(F32, [128, 512]) for _ in range(2)]
                lt = [A.alloc(BF16, [128, 512]) for _ in range(3)]
                Ls = [A.alloc(BF16, [128, 512]) for _ in range(2)]
                At = [A.alloc(BF16, [128, 512]) for _ in range(2)]
                osb = [A.alloc(BF16, [128, 512]) for _ in range(2)]
                stg2 = [A.alloc(F32, [128, 1024]) for _ in range(2)]
                gf1 = A.alloc(F32, [128, D])
                gf2 = A.alloc(F32, [128, D])
                wt2 = [A.alloc(BF16, [128, D]) for _ in range(2)]
                P.dma(gf1, modrow(l, 2))
                P.dma(gf2, modrow(l, 5))
                wu_pieces = [(k, c0) for k in range(8) for c0 in range(0, DFF, 704)]
                for pt in range(4):
                    for j in range(3):
                        c0 = 1808 + j * 512 + pt * 128
                        sb_ = stg2[cast_ctr[0] % 2]
                        cast_ctr[0] += 1
                        P.dma(sb_[:, 0:1024].rearrange("p (k n) -> p k n", k=8),
                              win_d[l][:, c0:c0 + 128].rearrange("(k p) n -> p k n", p=128))
                        P.copy(wsb[:, :, j * 128:(j + 1) * 128], sb_[:, 0:1024].rearrange("p (k n) -> p k n", k=8))
                    for (k_, c0_) in wu_pieces[pt * 8:(pt + 1) * 8]:
                        load_cast(wus[:, k_, c0_:c0_ + 704], wu_d[l][k_ * 128:(k_ + 1) * 128, c0_:c0_ + 704], stg2)
                    for k_ in (2 * pt, 2 * pt + 1):
                        load_cast(wo[:, k_, :], wout_d[l][k_ * 128:(k_ + 1) * 128, :], stg2, fold=gf1)
                    for f_ in range(6 * pt, min(NF, 6 * pt + 6)):
                        load_cast(wt2[f_ % 2], wd_d[l][f_ * 128:(f_ + 1) * 128, :], stg2, fold=gf2)
                        P.dma(wd2_d[l][f_ * 128:(f_ + 1) * 128, :], wt2[f_ % 2])
                    for tc in range(NQC):
                        hc = hTc[0]
                        P.dma(hc, hT_d[:, tc * 512:(tc + 1) * 512].rearrange("(k p) t -> p k t", p=128))
                        pq, pk, pv = psb(0), psb(1), psb(2)
                        for k in range(8):
                            P.mm(pq, wsb[:, k, 0:128], hc[:, k, :], start=(k == 0), stop=(k == 7))
                        for k in range(8):
                            P.mm(pk, wsb[:, k, 128:256], hc[:, k, :], start=(k == 0), stop=(k == 7))
                        for t4 in range(4):
                            for k in range(8):
                                P.mm(pv[:, t4 * 128:(t4 + 1) * 128], hc[:, k, t4 * 128:(t4 + 1) * 128],
                                     wsb[:, k, 256:384], start=(k == 0), stop=(k == 7))
                        P.act(QT[:, tc * 512:(tc + 1) * 512], pq, AF.Copy, scale=0.125)
                        P.copy(KTz[0][0:64, tc * 512:(tc + 1) * 512], pk[0:64, :])
                        P.copy(KTz[1][64:128, tc * 512:(tc + 1) * 512], pk[64:128, :])
                        P.copy(Vp[:, tc * 4:(tc + 1) * 4, :].rearrange("p a b -> p (a b)"), pv, eng="act")
                    tiles = []
                    for hh in range(2):
                        for qc in range(NQC):
                            kbs = list(range(4 * qc + 3, -1, -1))
                            for ii, kb in enumerate(kbs):
                                tiles.append((hh, qc, kb, ii == 0, ii == len(kbs) - 1))
                    zb = [psb(3), psb(4), psb(5)]
                    ob = [psb(6), psb(7)]

                    def stageA(i):
                        hh, qc, kb, first, last = tiles[i]
                        r0 = hh * 64
                        t0 = qc * 512
                        j = max(0, kb - 4 * qc)
                        q0 = j * 128
                        z = zb[i % 3]
                        diag = kb >= 4 * qc
                        P.mm(z[:, q0:512], KTz[hh][:, kb * 128:(kb + 1) * 128], QT[:, t0 + q0:t0 + 512],
                             start=True, stop=not diag)
                        if diag:
                            P.mm(z[:, q0:q0 + 128], ident_bf, negstrict, start=False, stop=True)
                        e = ez[i % 2]
                        P.act(e[:, q0:512], z[:, q0:512], AF.Exp)
                        P.act(lt[i % 3][:, q0:512], e[:, q0:512], AF.Ln, bias=1.0)

                    def stageB(i):
                        hh, qc, kb, first, last = tiles[i]
                        t0 = qc * 512
                        j = max(0, kb - 4 * qc)
                        q0 = j * 128
                        z = zb[i % 3]
                        Lq = Ls[(hh * NQC + qc) % 2]
                        o = ob[(hh * NQC + qc) % 2]
                        l_ = lt[i % 3]
                        P.mm(z[:, q0:512], ntri, l_[:, q0:512], start=False, stop=first, skip_group_check=True)
                        if not first:
                            P.mm(z[:, q0:512], nones, Lq[:, q0:512], start=False, stop=True, skip_group_check=True)
                        a = At[i % 2]
                        P.act(a[:, q0:512], z[:, q0:512], AF.Exp)
                        P.mm(o[:, q0:512], Vp[:, kb, :], a[:, q0:512],
                             start=first, stop=last, skip_group_check=True)
                        if not last:
                            if first:
                                if q0 > 0:
                                    P.memset(Lq[:, 0:q0], 0.0)
                                P.copy(Lq[:, q0:512], l_[:, q0:512])
                            else:
                                if q0 > 0:
                                    pass
                                P.tt(Lq[:, q0:512], Lq[:, q0:512], l_[:, q0:512], ALU.add)
                        if last:
                            so = osb[(hh * NQC + qc) % 2]
                            r0 = hh * 64
                            P.copy(so[r0:r0 + 64, :], o[r0:r0 + 64, :])
                            e0 = 512 + (pt * 2 + hh) * 64
                            P.dma(oT_d[e0:e0 + 64, t0:t0 + 512], so[r0:r0 + 64, :])

                    def keep_warm(k):
                        for _ in range(k):
                            P.mm(psb(0), nones, warm_rhs, start=True, stop=True)

                    n = len(tiles)
                    for _ in range(14):
                        P.mm(psb(0), nones, warm_rhs, start=True, stop=True)
                    stageA(0)
                    for i in range(n):
                        if i + 1 < n:
                            stageA(i + 1)
                        keep_warm(1)
                        stageB(i)
                        keep_warm(2)
                A.release(m2)

            if 3 in phases:
                m3 = A.mark()
                a2b = A.alloc(F32, [128, D])
                sh2b = A.alloc(F32, [128, D])
                P.dma(a2b, modrow(l, 4))
                P.dma(sh2b, modrow(l, 3))
                if last_layer:
                    fgb = A.alloc(F32, [128, D])
                    P.dma(fgb, fg_d.to_broadcast([128, D]))
                wdbuf = [A.alloc(BF16, [128, D]) for _ in range(4)]
                TC = 256
                NT = TC // 128
                oTc = [A.alloc(BF16, [128, 8, TC]) for _ in range(2)]
                x0 = [A.alloc(F32, [128, D]) for _ in range(2)]
                x1 = [A.alloc(F32, [128, D]) for _ in range(NT)]
                scr = {"junk": A.alloc(BF16, [128, D]), "ss": A.alloc(F32, [128, 1]),
                       "ln": A.alloc(F32, [128, 1]), "rstd": A.alloc(F32, [128, 1]),
                       "xm": A.alloc(F32, [128, D]), "xn": A.alloc(BF16, [128, D]), "eps": epsc}
                h2T = A.alloc(BF16, [128, 8, TC])
                actT = A.alloc(BF16, [128, NF, TC])
                ea = [A.alloc(F32, [128, TC]) for _ in range(2)]
                sa = [A.alloc(F32, [128, TC]) for _ in range(2)]
                for c in range(S // TC):
                    oc = oTc[c % 2]
                    P.dma(oc, oT_d[:, c * TC:(c + 1) * TC].rearrange("(k p) t -> p k t", p=128))
                    for tbl in range(NT):
                        tb = c * NT + tbl
                        xb = x0[tbl % 2]
                        P.dma(xb, xsrc[tb * 128:(tb + 1) * 128, :])
                        for hf in range(2):
                            ps = psb(hf)
                            for e in range(8):
                                P.mm(ps, oc[:, e, tbl * 128:(tbl + 1) * 128], wo[:, e, hf * 512:(hf + 1) * 512],
                                     start=(e == 0), stop=(e == 7))
                            P.tt(x1[tbl][:, hf * 512:(hf + 1) * 512], ps, xb[:, hf * 512:(hf + 1) * 512], ALU.add)
                        if dbg and l == 0:
                            P.dma(dbg_d["d_x1"][tb * 128:(tb + 1) * 128, :], x1[tbl])
                        norm_block(x1[tbl], a2b, sh2b, scr, h2T[:, :, tbl * 128:(tbl + 1) * 128], psb(2, BF16))
                    for f in range(NF):
                        pa, pb = psb(3 + 2 * (f % 2)), psb(4 + 2 * (f % 2))
                        for k in range(8):
                            P.mm(pa[:, 0:TC], wgs[:, k, f * 128:(f + 1) * 128], h2T[:, k, :], start=(k == 0), stop=(k == 7))
                        for k in range(8):
                            P.mm(pb[:, 0:TC], wus[:, k, f * 128:(f + 1) * 128], h2T[:, k, :], start=(k == 0), stop=(k == 7))
                        e_ = ea[f % 2]
                        s_ = sa[f % 2]
                        P.act(e_, pa[:, 0:TC], AF.Exp, scale=-1.0)
                        P.act(e_, e_, AF.Ln, bias=1.0)
                        P.act(e_, e_, AF.Exp, scale=-1.0)
                        P.tt(s_, pa[:, 0:TC], e_, ALU.mult)
                        P.tt(actT[:, f, :], s_, pb[:, 0:TC], ALU.mult)
                    accb = [psb(0), psb(1), psb(2), psb(7)]
                    for f in range(NF):
                        wdb = wdbuf[f % 4]
                        P.dma(wdb, wd2_d[l][f * 128:(f + 1) * 128, :])
                        for tbl in range(NT):
                            for hf in range(2):
                                P.mm(accb[tbl * 2 + hf], actT[:, f, tbl * 128:(tbl + 1) * 128],
                                     wdb[:, hf * 512:(hf + 1) * 512], start=(f == 0), stop=(f == NF - 1))
                    for tbl in range(NT):
                        tb = c * NT + tbl
                        xo_ = x1[tbl]
                        for hf in range(2):
                            P.tt(xo_[:, hf * 512:(hf + 1) * 512], accb[tbl * 2 + hf],
                                 x1[tbl][:, hf * 512:(hf + 1) * 512], ALU.add)
                        if not last_layer:
                            P.dma(xs_d[tb * 128:(tb + 1) * 128, :], xo_)
                        else:
                            P.act(scr["junk"], xo_, AF.Square, accum_out=scr["ss"])
                            P.act(scr["ln"], scr["ss"], AF.Ln, scale=1.0 / D, bias=epsc)
                            P.act(scr["rstd"], scr["ln"], AF.Exp, scale=-0.5)
                            P.stt(scr["xm"], xo_, scr["rstd"], fgb, ALU.mult, ALU.mult)
                            P.dma(out_d[tb * 128:(tb + 1) * 128, :], scr["xm"])
                A.release(m3)
            A.release(ml)
            if dbg and l == 0:
                P.barrier()
                for k in range(8):
                    P.dma(dbg_d["d_hT"][k * 128:(k + 1) * 128, :], hT_d[k * 128:(k + 1) * 128, :])
                    P.dma(dbg_d["d_oT"][k * 128:(k + 1) * 128, :], oT_d[k * 128:(k + 1) * 128, :])
                P.barrier()
        P.barrier()
        P.emit()
    return nc, cst


_CACHE = {}


def _get(S):
    if S not in _CACHE:
        _CACHE[S] = build(S)
    return _CACHE[S]


def make_in_maps(inputs, cst, S, ncores):
    maps = []
    shared = {}
    for k in ("ada_w", "w_in", "gla_wa2", "w_out", "ffn_wg", "ffn_wu", "ffn_wd"):
        shared[k] = np.ascontiguousarray(np.asarray(inputs[k], dtype=np.float32))
    for k in ("ada_b", "norm1_g", "norm2_g", "gla_ba", "gla_norm_g", "ret_norm_g"):
        a = np.asarray(inputs[k], dtype=np.float32)
        shared[k] = np.ascontiguousarray(a.reshape(a.shape[0], 1, a.shape[1]))
    shared["final_g"] = np.ascontiguousarray(np.asarray(inputs["final_g"], dtype=np.float32).reshape(1, D))
    for k, v in cst.items():
        shared["c_" + k] = np.ascontiguousarray(v)
    x = np.asarray(inputs["x"], dtype=np.float32)
    c = np.asarray(inputs["c"], dtype=np.float32)
    for b in range(ncores):
        m = dict(shared)
        m["x"] = np.ascontiguousarray(x[b])
        m["cT"] = np.ascontiguousarray(c[b].reshape(8, 128).T)
        maps.append(m)
    return maps


def kernel(**inputs):
    x = np.asarray(inputs["x"])
    B, S, _ = x.shape
    nc, cst = _get(S)
    maps = make_in_maps(inputs, cst, S, B)
    res = run_bass_kernel_spmd(nc, maps, core_ids=list(range(B)))
    out = np.stack([np.asarray(res.results[b]["out"], dtype=np.float32) for b in range(B)], axis=0)
    return out
```

```python
import contextlib
import numpy as np
import ml_dtypes
import concourse.bass as bass
import concourse.mybir as mybir
from concourse.bass_utils import run_bass_kernel_spmd

F32 = mybir.dt.float32
BF16 = mybir.dt.bfloat16
U8 = mybir.dt.uint8
AF = mybir.ActivationFunctionType
ALU = mybir.AluOpType
AX = mybir.AxisListType
_ISZ = {F32: 4, BF16: 2, U8: 1}

D = 1024
DEPTH = 2
DFF = 2816
NF = DFF // 128
INC = 3344
EPS = 1e-6
NEG = -30000.0


class _Op:
    __slots__ = ("eng", "fn", "deps", "sig", "sigval", "dsem", "dval", "isdma", "gi")

    def __init__(self, eng, fn, isdma, gi):
        self.eng = eng
        self.fn = fn
        self.deps = set()
        self.sig = False
        self.sigval = 0
        self.dsem = -1
        self.dval = 0
        self.isdma = isdma
        self.gi = gi


class Prog:
    ENGS = ("pe", "act", "dve", "pool", "sp")
    NDMA = 40

    def __init__(self, nc, ro_names=()):
        self.nc = nc
        self.ops = []
        self.track = {}
        self.ro = set(ro_names)
        self.dma_rr = 0
        self.dma_last = [None] * self.NDMA
        self.dma_cnt = [0] * self.NDMA
        self.last_on = {e: None for e in self.ENGS}

    def _box(self, ap):
        t = ap.tensor
        ps = 1
        for s in list(t.shape)[1:]:
            ps *= int(s)
        isz = _ISZ[ap.dtype]
        off = int(ap.offset)
        p0 = off // ps
        f0 = off % ps
        pe = 0
        fe = 0
        for (st, cnt) in ap.ap:
            st = int(st)
            cnt = int(cnt)
            if cnt <= 1 or st == 0:
                continue
            if st % ps == 0:
                pe += (cnt - 1) * (st // ps)
            else:
                fe += (cnt - 1) * st
        if t.name == "PS":
            lo = (f0 * isz) // 2048 * 2048
            hi = -(-((f0 + fe + 1) * isz) // 2048) * 2048
            return (t.name, 0, 128, lo, hi)
        return (t.name, p0, p0 + pe + 1, f0 * isz, (f0 + fe + 1) * isz)

    def _access(self, op, ap, is_write):
        name, p0, p1, b0, b1 = self._box(ap)
        if name in self.ro:
            return
        d = self.track.setdefault(name, {})
        ekey = ("dma", op.gi) if op.isdma else op.eng
        dead = []
        for key, oi in d.items():
            (w, ek, q0, q1, c0, c1) = key
            if not (w or is_write):
                continue
            if q1 <= p0 or p1 <= q0 or c1 <= b0 or b1 <= c0:
                continue
            if oi == op.gi:
                continue
            if ek == ekey and op.eng == "pe":
                pass
            else:
                op.deps.add(oi)
            if is_write and q0 >= p0 and q1 <= p1 and c0 >= b0 and c1 <= b1:
                dead.append(key)
        for k in dead:
            del d[k]
        d[(is_write, ekey, p0, p1, b0, b1)] = op.gi

    def add(self, eng, fn, reads=(), writes=(), isdma=False):
        op = _Op(eng, fn, isdma, len(self.ops))
        self.ops.append(op)
        for ap in reads:
            if ap is not None and not isinstance(ap, (int, float)):
                self._access(op, ap, False)
        for ap in writes:
            self._access(op, ap, True)
        if isdma:
            k = self.dma_rr
            self.dma_rr = (self.dma_rr + 1) % self.NDMA
            if self.dma_last[k] is not None:
                op.deps.add(self.dma_last[k])
            self.dma_last[k] = op.gi
            self.dma_cnt[k] += 16
            op.dsem = k
            op.dval = self.dma_cnt[k]
            op.sig = True
        op.deps.discard(op.gi)
        for dgi in op.deps:
            self.ops[dgi].sig = True
        self.last_on[eng] = op.gi
        return op

    def barrier(self):
        lasts = [gi for gi in self.last_on.values() if gi is not None]
        dmas = [gi for gi in self.dma_last if gi is not None]
        for e in self.ENGS:
            op = _Op(e, None, False, len(self.ops))
            self.ops.append(op)
            for gi in lasts + dmas:
                o = self.ops[gi]
                if o.fn is None or (o.eng == e and not o.isdma):
                    continue
                op.deps.add(gi)
                o.sig = True

    def mm(self, out, lhsT, rhs, start=True, stop=True, **kw):
        return self.add("pe", lambda e: e.matmul(out, lhsT, rhs, start=start, stop=stop, **kw),
                        reads=[lhsT, rhs], writes=[out])

    def transpose(self, out, in_, ident):
        return self.add("pe", lambda e: e.transpose(out, in_, ident), reads=[in_, ident], writes=[out])

    def act(self, out, in_, func, bias=None, scale=None, accum_out=None):
        kw = {}
        rd = [in_]
        wr = [out]
        if bias is not None:
            kw["bias"] = bias
            rd.append(bias)
        if scale is not None:
            kw["scale"] = scale
            rd.append(scale)
        if accum_out is not None:
            kw["accum_out"] = accum_out
            wr.append(accum_out)
        return self.add("act", lambda e: e.activation(out, in_, func, **kw), reads=rd, writes=wr)

    def tt(self, out, in0, in1, op, eng="dve"):
        return self.add(eng, lambda e: e.tensor_tensor(out, in0, in1, op), reads=[in0, in1], writes=[out])

    def ts(self, out, in0, s1, s2, op0, op1=None, eng="dve"):
        kw = {}
        if op1 is not None:
            kw["op1"] = op1
        return self.add(eng, lambda e: e.tensor_scalar(out, in0, s1, s2, op0, **kw),
                        reads=[in0, s1, s2], writes=[out])

    def stt(self, out, in0, scalar, in1, op0, op1):
        return self.add("dve", lambda e: e.scalar_tensor_tensor(out, in0, scalar, in1, op0, op1),
                        reads=[in0, scalar, in1], writes=[out])

    def copy(self, out, in_, eng="dve"):
        if eng == "act":
            return self.add("act", lambda e: e.copy(out, in_), reads=[in_], writes=[out])
        return self.add(eng, lambda e: e.tensor_copy(out, in_), reads=[in_], writes=[out])

    def memset(self, ap, val, eng="dve"):
        return self.add(eng, lambda e: e.memset(ap, val), writes=[ap])

    def reduce(self, out, in_, axis, op):
        return self.add("dve", lambda e: e.tensor_reduce(out, in_, axis, op), reads=[in_], writes=[out])

    def recip(self, out, in_):
        return self.add("dve", lambda e: e.reciprocal(out, in_), reads=[in_], writes=[out])

    def dma(self, out, in_, eng="sp", **kw):
        return self.add(eng, lambda e: e.dma_start(out=out, in_=in_, **kw), reads=[in_], writes=[out],
                        isdma=True)

    def emit(self):
        nc = self.nc
        cnt = {e: 0 for e in self.ENGS}
        for op in self.ops:
            if op.isdma or op.fn is None:
                continue
            if op.sig:
                cnt[op.eng] += 1
                op.sigval = cnt[op.eng]
        with contextlib.ExitStack() as st:
            esem = {e: st.enter_context(nc.semaphore("es_" + e)) for e in self.ENGS}
            dsem = [st.enter_context(nc.semaphore("ds%d" % i)) for i in range(self.NDMA)]
            block = st.enter_context(nc.Block())
            handles = {"pe": block.tensor, "act": block.scalar, "dve": block.vector,
                       "pool": block.gpsimd, "sp": block.sync}
            ops = self.ops

            def make(ename):
                def body(eng):
                    seen = {}
                    for op in ops:
                        if op.eng != ename:
                            continue
                        need = {}
                        for dgi in op.deps:
                            d = ops[dgi]
                            if d.isdma:
                                key = ("d", d.dsem)
                                val = d.dval
                            else:
                                if d.fn is None:
                                    continue
                                key = ("e", d.eng)
                                val = d.sigval
                            if val > need.get(key, 0):
                                need[key] = val
                        for key, val in need.items():
                            if seen.get(key, 0) >= val:
                                continue
                            seen[key] = val
                            sem = dsem[key[1]] if key[0] == "d" else esem[key[1]]
                            eng.wait_ge(sem, val)
                        if op.fn is None:
                            continue
                        inst = op.fn(eng)
                        if op.isdma:
                            inst.then_inc(dsem[op.dsem], 16)
                        elif op.sig:
                            inst.then_inc(esem[op.eng], 1)
                return body

            for ename in self.ENGS:
                handles[ename](make(ename))


class Arena:
    def __init__(self, ap_u8, size):
        self.t = ap_u8
        self.size = size
        self.top = 0
        self.peak = 0

    def mark(self):
        return self.top

    def release(self, m):
        self.top = m

    def alloc(self, dtype, shape, parts=None):
        n = 1
        for s in shape[1:]:
            n *= s
        nb = n * _ISZ[dtype]
        off = (self.top + 63) // 64 * 64
        assert off + nb <= self.size, ("arena overflow", off, nb, self.size)
        self.top = off + nb
        self.peak = max(self.peak, self.top)
        v = self.t[0:shape[0], off:off + nb].bitcast(dtype)
        if len(shape) == 3:
            v = v.rearrange("p (a b) -> p a b", b=shape[2])
        elif len(shape) == 4:
            v = v.rearrange("p (a b c) -> p a b c", b=shape[2], c=shape[3])
        return v


def _consts(S):
    NB = S // 128
    c = {}
    idx = np.arange(128)
    c["ident"] = np.eye(128, dtype=np.float32)
    c["m_incl"] = (idx[:, None] <= idx[None, :]).astype(np.float32)
    c["negstrict"] = np.where(idx[:, None] >= idx[None, :], NEG, 0.0).astype(np.float32)
    c["ntri"] = -(idx[:, None] >= idx[None, :]).astype(np.float32)
    c["nones"] = -np.ones((128, 128), np.float32)
    c["trig"] = c["m_incl"] * (-1.0 / 16.0)
    c["n16"] = np.full((128, 1), -1.0 / 16.0, np.float32)
    inv = 10000.0 ** (-np.arange(0, 64, 2, dtype=np.float32) / 64.0)
    pos = np.arange(S, dtype=np.float32)
    ang = pos[:, None] * inv[None, :]
    c["cos"] = np.cos(ang).astype(np.float32).reshape(NB, 128, 32).transpose(1, 0, 2).copy()
    c["sin"] = np.sin(ang).astype(np.float32).reshape(NB, 128, 32).transpose(1, 0, 2).copy()
    gam = 1.0 - 2.0 ** (-5.0 - np.arange(4, dtype=np.float64))
    tl = np.arange(128, dtype=np.float64)
    qs = gam[None, :] ** (tl[:, None] + 1.0)
    ks = gam[None, :] ** (-(tl[:, None] + 1.0)) / 8.0
    c["rsc"] = np.concatenate([qs, ks], axis=1).astype(np.float32)
    gc = gam ** 128.0
    c["retD"] = np.stack([np.repeat(gc[0:2], 64), np.repeat(gc[2:4], 64)], axis=1).astype(np.float32)
    bm = np.zeros((128, 256), np.float32)
    for h in range(4):
        bm[32 * h:32 * h + 32, 64 * h:64 * h + 64] = 1.0
    c["bm_gla"] = bm
    bm2 = np.zeros((128, 128), np.float32)
    for h in range(2):
        bm2[64 * h:64 * h + 64, 64 * h:64 * h + 64] = 1.0
    c["bm_ret"] = bm2
    c["onerow"] = np.ones((1, 128), np.float32)
    return c


_CONST_SHAPES = None


def build(S, dbg=False, nlayers=DEPTH, phases=(0, 1, 2, 3)):
    NB = S // 128
    NQC = S // 512
    nc = bass.Bass("TRN2", target_bir_lowering=False)
    cst = _consts(S)
    ins = {}

    def din(name, shape, dt=F32):
        ins[name] = nc.dram_tensor(name, list(shape), dt, kind="ExternalInput").ap()
        return ins[name]

    x_d = din("x", [S, D])
    cT_d = din("cT", [128, 8])
    adaw_d = din("ada_w", [DEPTH, D, 6 * D])
    adab_d = din("ada_b", [DEPTH, 1, 6 * D])
    n1g_d = din("norm1_g", [DEPTH, 1, D])
    n2g_d = din("norm2_g", [DEPTH, 1, D])
    win_d = din("w_in", [DEPTH, D, INC])
    wa2_d = din("gla_wa2", [DEPTH, 16, 128])
    gba_d = din("gla_ba", [DEPTH, 1, 128])
    glag_d = din("gla_norm_g", [DEPTH, 1, 64])
    retg_d = din("ret_norm_g", [DEPTH, 1, 64])
    wout_d = din("w_out", [DEPTH, D, D])
    wg_d = din("ffn_wg", [DEPTH, D, DFF])
    wu_d = din("ffn_wu", [DEPTH, D, DFF])
    wd_d = din("ffn_wd", [DEPTH, DFF, D])
    fg_d = din("final_g", [1, D])
    cd = {k: din("c_" + k, v.shape) for k, v in cst.items()}
    out_d = nc.dram_tensor("out", [S, D], F32, kind="ExternalOutput").ap()
    xs_d = nc.dram_tensor("xs", [S, D], F32, kind="Internal").ap()
    hT_d = nc.dram_tensor("hT", [D, S], BF16, kind="Internal").ap()
    oT_d = nc.dram_tensor("oT", [D, S], BF16, kind="Internal").ap()
    mod_d = nc.dram_tensor("modrow", [DEPTH, 1, 6 * D], F32, kind="Internal").ap()
    wd2_d = nc.dram_tensor("wd2", [DEPTH, DFF, D], BF16, kind="Internal").ap()
    dbg_d = {}
    if dbg:
        dbg_d["d_hT"] = nc.dram_tensor("d_hT", [D, S], BF16, kind="ExternalOutput").ap()
        dbg_d["d_oT"] = nc.dram_tensor("d_oT", [D, S], BF16, kind="ExternalOutput").ap()
        dbg_d["d_mod"] = nc.dram_tensor("d_mod", [DEPTH, 6, D], F32, kind="ExternalOutput").ap()
        dbg_d["d_x1"] = nc.dram_tensor("d_x1", [S, D], F32, kind="ExternalOutput").ap()

    P = Prog(nc, ro_names=list(ins.keys()))
    SBYTES = 207 * 1024
    with contextlib.ExitStack() as st:
        SBt = st.enter_context(nc.sbuf_tensor("SB", [128, SBYTES], U8))
        PSt = st.enter_context(nc.psum_tensor("PS", [128, 16384], U8))
        A = Arena(SBt, SBYTES)

        def psb(b, dt=F32):
            return PSt[:, b * 2048:(b + 1) * 2048].bitcast(dt)

        ident_bf = A.alloc(BF16, [128, 128])
        ident_f = A.alloc(F32, [128, 128])
        m_incl = A.alloc(F32, [128, 128])
        negstrict = A.alloc(BF16, [128, 128])
        ntri = A.alloc(BF16, [128, 128])
        nones = A.alloc(BF16, [128, 128])
        trig = A.alloc(F32, [128, 128])
        n16 = A.alloc(F32, [128, 1])
        cos_t = A.alloc(F32, [128, NB, 32])
        sin_t = A.alloc(F32, [128, NB, 32])
        rsc = A.alloc(F32, [128, 8])
        retD = A.alloc(F32, [128, 2])
        bm_gla = A.alloc(F32, [128, 256])
        bm_ret = A.alloc(F32, [128, 128])
        onerow = A.alloc(F32, [1, 128])
        one11 = onerow[0:1, 0:1]
        mc = A.mark()
        for dst, nm in ((ident_bf, "ident"), (negstrict, "negstrict"), (ntri, "ntri"), (nones, "nones")):
            cs_ = A.alloc(F32, [128, 128])
            P.dma(cs_, cd[nm])
            P.copy(dst, cs_)
        A.release(mc)
        P.dma(ident_f, cd["ident"])
        P.dma(m_incl, cd["m_incl"])
        P.dma(trig, cd["trig"])
        P.dma(n16, cd["n16"])
        P.dma(cos_t, cd["cos"])
        P.dma(sin_t, cd["sin"])
        P.dma(rsc, cd["rsc"])
        P.dma(retD, cd["retD"])
        P.dma(bm_gla, cd["bm_gla"])
        P.dma(bm_ret, cd["bm_ret"])
        P.dma(onerow, cd["onerow"])
        epsc = A.alloc(F32, [128, 1])
        P.memset(epsc, EPS)
        warm_rhs = A.alloc(BF16, [128, 512])
        P.memset(warm_rhs, 0.0)

        m0 = A.mark()
        cT = A.alloc(F32, [128, 8])
        csT = A.alloc(F32, [128, 8])
        tmp8 = A.alloc(F32, [128, 8])
        row = A.alloc(F32, [1, 6 * D])
        grow = A.alloc(F32, [1, 2 * D + 128])
        brow = A.alloc(F32, [1, 6 * D])
        wbuf = [A.alloc(F32, [128, 8, 512]) for _ in range(2)]
        P.dma(cT, cT_d)
        P.act(tmp8, cT, AF.Exp, scale=-1.0)
        P.ts(tmp8, tmp8, 1.0, None, ALU.add)
        P.recip(tmp8, tmp8)
        P.tt(csT, cT, tmp8, ALU.mult)
        for l in range(nlayers):
            P.dma(brow, adab_d[l])
            for cg in range(12):
                wb = wbuf[cg % 2]
                P.dma(wb, adaw_d[l][:, cg * 512:(cg + 1) * 512].rearrange("(k p) n -> p k n", p=128),
                      eng="sp")
                ps = psb(cg % 2)
                for k in range(8):
                    P.mm(ps[0:1, :], csT[:, k:k + 1], wb[:, k, :], start=(k == 0), stop=(k == 7))
                P.tt(row[0:1, cg * 512:(cg + 1) * 512], ps[0:1, :], brow[0:1, cg * 512:(cg + 1) * 512], ALU.add)
            P.dma(grow[0:1, 0:D], n1g_d[l])
            P.dma(grow[0:1, D:2 * D], n2g_d[l])
            P.stt(row[0:1, D:2 * D], row[0:1, D:2 * D], 1.0, grow[0:1, 0:D], ALU.add, ALU.mult)
            P.stt(row[0:1, 4 * D:5 * D], row[0:1, 4 * D:5 * D], 1.0, grow[0:1, D:2 * D], ALU.add, ALU.mult)
            if dbg:
                P.dma(dbg_d["d_mod"][l:l + 1].rearrange("o s d -> o (s d)"), row)
            P.dma(mod_d[l], row)
        A.release(m0)

        def norm_block(xb, ab, shb, scr, hTb, psT):
            P.act(scr["junk"], xb, AF.Square, accum_out=scr["ss"])
            P.act(scr["ln"], scr["ss"], AF.Ln, scale=1.0 / D, bias=scr["eps"])
            P.act(scr["rstd"], scr["ln"], AF.Exp, scale=-0.5)
            P.stt(scr["xm"], xb, scr["rstd"], ab, ALU.mult, ALU.mult)
            P.tt(scr["xn"], scr["xm"], shb, ALU.add)
            for k in range(8):
                P.transpose(psT[:, k * 128:(k + 1) * 128], scr["xn"][:, k * 128:(k + 1) * 128], ident_bf)
            P.copy(hTb, psT.rearrange("p (a b) -> p a b", a=8), eng="act")

        def modrow(l, j):
            return mod_d[l][0:1, j * D:(j + 1) * D].to_broadcast([128, D])

        for l in range(nlayers):
            xsrc = x_d if l == 0 else xs_d
            last_layer = (l == nlayers - 1)
            ml = A.mark()
            wgs = A.alloc(BF16, [128, 8, DFF])
            cast_ctr = [0]

            def load_cast(dst, src, stg, fold=None):
                sb_ = stg[cast_ctr[0] % len(stg)]
                cast_ctr[0] += 1
                n = dst.shape[-1]
                P.dma(sb_[:, 0:n], src)
                if fold is not None:
                    P.tt(dst, sb_[:, 0:n], fold, ALU.mult)
                elif cast_ctr[0] % 2 == 0:
                    P.copy(dst, sb_[:, 0:n], eng="act")
                else:
                    P.copy(dst, sb_[:, 0:n])
            if 1 in phases:
                m1 = A.mark()
                a1b = A.alloc(F32, [128, D])
                sh1b = A.alloc(F32, [128, D])
                gnb_l = A.alloc(F32, [128, 512])
                wa2_l = A.alloc(F32, [16, 128])
                gba_l = A.alloc(F32, [1, 128])
                P.dma(a1b, modrow(l, 1))
                P.dma(sh1b, modrow(l, 0))
                for r in range(8):
                    srcg = glag_d[l] if r < 4 else retg_d[l]
                    P.dma(gnb_l[:, r * 64:(r + 1) * 64], srcg.to_broadcast([128, 64]))
                P.dma(wa2_l, wa2_d[l])
                P.dma(gba_l, gba_d[l])
                wla = A.alloc(BF16, [128, 8, 1808])
                stg1 = [A.alloc(F32, [128, 1408]) for _ in range(2)]
                for k in range(8):
                    for c0 in (0, 904):
                        load_cast(wla[:, k, c0:c0 + 904], win_d[l][k * 128:(k + 1) * 128, c0:c0 + 904], stg1)
                xbs = [A.alloc(F32, [128, D]) for _ in range(2)]
                scr = {"junk": A.alloc(BF16, [128, D]), "ss": A.alloc(F32, [128, 1]),
                       "ln": A.alloc(F32, [128, 1]), "rstd": A.alloc(F32, [128, 1]),
                       "xm": A.alloc(F32, [128, D]), "xn": A.alloc(BF16, [128, D]), "eps": epsc}
                hTbs = [A.alloc(BF16, [128, 8, 128]) for _ in range(2)]
                grT = A.alloc(F32, [16, 128])
                e1 = A.alloc(F32, [128, 128])
                lsp = A.alloc(F32, [128, 128])
                eb = A.alloc(F32, [128, 128])
                enb = A.alloc(F32, [128, 128])
                Dg = A.alloc(F32, [128, 1])
                qg = A.alloc(BF16, [128, 128])
                kg = A.alloc(BF16, [128, 128])
                KX = A.alloc(BF16, [128, 640])
                KXr = A.alloc(BF16, [128, 4, 128])
                rt = [A.alloc(F32, [128, 8, 32]) for _ in range(4)]
                qkr32 = A.alloc(F32, [128, 8, 32, 2])
                QKr = A.alloc(BF16, [128, 512])
                qkT = A.alloc(BF16, [128, 11, 128])
                scT = A.alloc(BF16, [128, 8, 128])
                Vall = A.alloc(BF16, [128, 512])
                S32 = [A.alloc(F32, [128, 256]), A.alloc(F32, [128, 128]), A.alloc(F32, [128, 128])]
                tS = [A.alloc(F32, [128, 256]), A.alloc(F32, [128, 128]), A.alloc(F32, [128, 128])]
                Sbf = [A.alloc(BF16, [128, 256]), A.alloc(BF16, [128, 128]), A.alloc(BF16, [128, 128])]
                eg = A.alloc(F32, [128, 512])
                sg = A.alloc(F32, [128, 512])
                osq = A.alloc(F32, [128, 512])
                ssum = A.alloc(F32, [128, 8])
                orstd = A.alloc(F32, [128, 8])
                on = A.alloc(F32, [128, 512])
                og = A.alloc(BF16, [128, 512])
                oTb = A.alloc(BF16, [128, 4, 128])
                P.memset(KX, 0.0)
                P.memset(KXr, 0.0)
                for i in range(3):
                    P.memset(S32[i], 0.0)
                    P.memset(Sbf[i], 0.0)
                KXd = KX.rearrange("p (h c) -> p h c", c=160)[:, :, 0:32]
                b0, b1, b2, b3, b4 = psb(0), psb(1), psb(2), psb(3), psb(4)
                b5, b6, b7 = psb(5, BF16), psb(6, BF16), psb(7, BF16)
                wg_pieces = [(k, c0) for k in range(8) for c0 in range(0, DFF, 1408)]
                def norm_in(tb_):
                    norm_block(xbs[tb_ % 2], a1b, sh1b, scr, hTbs[tb_ % 2], b5)
                    P.dma(hT_d[:, tb_ * 128:(tb_ + 1) * 128].rearrange("(k p) t -> p k t", p=128), hTbs[tb_ % 2])

                for tb in range(NB):
                    hTb = hTbs[tb % 2]
                    P.dma(xbs[tb % 2], xsrc[tb * 128:(tb + 1) * 128, :])
                    norm_in(tb)
                    npp = -(-len(wg_pieces) // NB)
                    for (k_, c0_) in wg_pieces[tb * npp:(tb + 1) * npp]:
                        load_cast(wgs[:, k_, c0_:c0_ + 1408], wg_d[l][k_ * 128:(k_ + 1) * 128, c0_:c0_ + 1408], stg1)
                    for k in range(8):
                        P.mm(b0, hTb[:, k, :], wla[:, k, 0:512], start=(k == 0), stop=(k == 7))
                    for k in range(8):
                        P.mm(b1[:, 0:256], hTb[:, k, :], wla[:, k, 512:768], start=(k == 0), stop=(k == 7))
                    for k in range(8):
                        P.mm(b1[:, 256:384], wla[:, k, 768:896], hTb[:, k, :], start=(k == 0), stop=(k == 7))
                    for k in range(8):
                        P.mm(b2, hTb[:, k, :], wla[:, k, 784:1296], start=(k == 0), stop=(k == 7))
                    for k in range(8):
                        P.mm(b3, hTb[:, k, :], wla[:, k, 1296:1808], start=(k == 0), stop=(k == 7))
                    P.copy(grT, b1[0:16, 256:384])
                    P.copy(Vall[:, 0:256], b0[:, 256:512], eng="act")
                    P.copy(Vall[:, 256:512], b3[:, 0:256], eng="act")
                    P.act(eg[:, 0:256], b1[:, 0:256], AF.Exp, scale=-1.0)
                    P.act(eg[:, 256:512], b3[:, 256:512], AF.Exp, scale=-1.0)
                    P.act(eg, eg, AF.Ln, bias=1.0)
                    P.act(eg, eg, AF.Exp, scale=-1.0)
                    P.tt(sg[:, 0:256], b1[:, 0:256], eg[:, 0:256], ALU.mult)
                    P.tt(sg[:, 256:512], b3[:, 256:512], eg[:, 256:512], ALU.mult)
                    P.mm(b4[:, 0:128], grT, wa2_l, start=True, stop=False)
                    P.mm(b4[:, 0:128], onerow[0:1, :], gba_l, start=False, stop=True)
                    P.act(e1, b4[:, 0:128], AF.Exp, scale=-1.0)
                    P.act(lsp, e1, AF.Ln, bias=1.0)
                    P.mm(b4[:, 128:256], trig, lsp, start=True, stop=True)
                    P.mm(b4[:, 256:257], lsp, n16, start=True, stop=True)
                    P.act(eb, b4[:, 128:256], AF.Exp)
                    P.act(enb, b4[:, 128:256], AF.Exp, scale=-1.0)
                    P.act(Dg, b4[:, 256:257], AF.Exp)
                    P.stt(qg, b0[:, 0:128], 32.0 ** -0.5, eb, ALU.mult, ALU.mult)
                    P.tt(kg, b0[:, 128:256], enb, ALU.mult)
                    P.copy(KXd, kg.rearrange("p (h d) -> p h d", h=4), eng="act")
                    qk4 = b2.rearrange("p (h i two) -> p h i two", h=8, two=2)
                    ev, od = qk4[:, :, :, 0], qk4[:, :, :, 1]
                    cb = cos_t[:, tb, :].unsqueeze(1).broadcast_to([128, 8, 32])
                    sb_ = sin_t[:, tb, :].unsqueeze(1).broadcast_to([128, 8, 32])
                    P.tt(rt[0], ev, cb, ALU.mult)
                    P.tt(rt[1], od, sb_, ALU.mult)
                    P.tt(rt[2], ev, sb_, ALU.mult)
                    P.tt(rt[3], od, cb, ALU.mult)
                    P.tt(qkr32[:, :, :, 0], rt[0], rt[1], ALU.subtract)
                    P.tt(qkr32[:, :, :, 1], rt[2], rt[3], ALU.add)
                    P.tt(QKr.rearrange("p (h d) -> p h d", h=8), qkr32.rearrange("p h i two -> p h (i two)"),
                         rsc.unsqueeze(2).broadcast_to([128, 8, 64]), ALU.mult)
                    P.transpose(b6[:, 0:128], qg, ident_bf)
                    for h in range(4):
                        P.transpose(b6[:, (1 + h) * 128:(2 + h) * 128], KX[:, h * 128:(h + 1) * 128], ident_bf)
                    for h in range(4):
                        c0 = (h % 2) * 64
                        P.copy(KXr[:, h, c0:c0 + 64], QKr[:, 256 + h * 64:320 + h * 64], eng=("act" if h % 2 else "dve"))
                    for j in range(2):
                        P.transpose(b6[:, (5 + j) * 128:(6 + j) * 128], QKr[:, j * 128:(j + 1) * 128], ident_bf)
                    P.transpose(b6[:, 7 * 128:8 * 128], KXr[:, 0, :], ident_bf)
                    for h in range(1, 4):
                        P.transpose(b7[:, (h - 1) * 128:h * 128], KXr[:, h, :], ident_bf)
                    P.copy(qkT[:, 0:8, :], b6.rearrange("p (a b) -> p a b", a=8), eng="act")
                    P.copy(qkT[:, 8:11, :], b7[:, 0:384].rearrange("p (a b) -> p a b", a=3), eng="act")
                    for h in range(4):
                        P.mm(b0[:, h * 128:(h + 1) * 128], qkT[:, 1 + h, :], qkT[:, 0, :], start=True, stop=True)
                    for h in range(4):
                        P.mm(b2[:, h * 128:(h + 1) * 128], qkT[:, 7 + h, :], qkT[:, 5 + h // 2, :],
                             start=True, stop=True)
                    mb = m_incl.unsqueeze(1).broadcast_to([128, 4, 128])
                    P.tt(scT[:, 0:4, :], b0.rearrange("p (h t) -> p h t", h=4), mb, ALU.mult)
                    P.tt(scT[:, 4:8, :], b2.rearrange("p (h t) -> p h t", h=4), mb, ALU.mult)
                    for h in range(8):
                        if h < 4:
                            ql, sl = qkT[:, 0, :], Sbf[0][:, h * 64:(h + 1) * 64]
                        else:
                            g = (h - 4) // 2
                            ql, sl = qkT[:, 5 + g, :], Sbf[1 + g][:, ((h - 4) % 2) * 64:((h - 4) % 2 + 1) * 64]
                        P.mm(b1[:, h * 64:(h + 1) * 64], scT[:, h, :], Vall[:, h * 64:(h + 1) * 64],
                             start=True, stop=False)
                        P.mm(b1[:, h * 64:(h + 1) * 64], ql, sl, start=False, stop=True)
                    P.mm(b3[:, 0:256], kg, Vall[:, 0:256], start=True, stop=True)
                    P.mm(b3[:, 256:384], QKr[:, 256:384], Vall[:, 256:384], start=True, stop=True)
                    P.mm(b3[:, 384:512], QKr[:, 384:512], Vall[:, 384:512], start=True, stop=True)
                    P.tt(tS[0], b3[:, 0:256], S32[0], ALU.add)
                    P.stt(S32[0], tS[0], Dg, bm_gla, ALU.mult, ALU.mult)
                    P.copy(Sbf[0], S32[0], eng="act")
                    for g in range(2):
                        P.tt(tS[1 + g], b3[:, 256 + g * 128:384 + g * 128], S32[1 + g], ALU.add)
                        P.stt(S32[1 + g], tS[1 + g], retD[:, g:g + 1], bm_ret, ALU.mult, ALU.mult)
                        P.copy(Sbf[1 + g], S32[1 + g], eng="act")
                    P.act(osq, b1, AF.Square)
                    P.reduce(ssum, osq.rearrange("p (h v) -> p h v", h=8), AX.X, ALU.add)
                    P.act(orstd, ssum, AF.Ln, scale=1.0 / 64.0, bias=epsc)
                    P.act(orstd, orstd, AF.Exp, scale=-0.5)
                    P.tt(on.rearrange("p (h v) -> p h v", h=8), b1.rearrange("p (h v) -> p h v", h=8),
                         orstd.unsqueeze(2).broadcast_to([128, 8, 64]), ALU.mult)
                    P.tt(on, on, gnb_l, ALU.mult)
                    P.tt(og, on, sg, ALU.mult)
                    for j in range(4):
                        P.transpose(b7[:, (3 + j) * 128:(4 + j) * 128], og[:, j * 128:(j + 1) * 128], ident_bf)
                    P.copy(oTb, b7[:, 384:896].rearrange("p (a b) -> p a b", a=4), eng="act")
                    P.dma(oT_d[0:512, tb * 128:(tb + 1) * 128].rearrange("(k p) t -> p k t", p=128), oTb)
                A.release(m1)

            wus = A.alloc(BF16, [128, 8, DFF])
            wo = A.alloc(BF16, [128, 8, D])
            if 2 in phases:
                m2 = A.mark()
                wsb = A.alloc(BF16, [128, 8, 384])
                hTc = [A.alloc(BF16, [128, 8, 512]) for _ in range(2)]
                QT = A.alloc(BF16, [128, S])
                KTz = [A.alloc(BF16, [128, S]) for _ in range(2)]
                P.memset(KTz[0][64:128, :], 0.0)
                P.memset(KTz[1][0:64, :], 0.0)
                Vp = A.alloc(BF16, [128, NB, 128])
                ez = [A.alloc(F32, [128, 512]) for _ in range(1)]
                lt = [A.alloc(BF16, [128, 512]) for _ in range(3)]
                Ls = [A.alloc(BF16, [128, 512]) for _ in range(2)]
                At = [A.alloc(BF16, [128, 512]) for _ in range(2)]
                osb = [A.alloc(BF16, [128, 512]) for _ in range(2)]
                stg2 = [A.alloc(F32, [128, 1024]) for _ in range(2)]
                gf = A.alloc(F32, [128, D])
                wt2 = [A.alloc(BF16, [128, D]) for _ in range(2)]
                P.dma(gf, modrow(l, 2))
                wu_pieces = [(k, c0) for k in range(8) for c0 in range(0, DFF, 704)]
                for pt in range(4):
                    for j in range(3):
                        c0 = 1808 + j * 512 + pt * 128
                        sb_ = stg2[cast_ctr[0] % 2]
                        cast_ctr[0] += 1
                        P.dma(sb_[:, 0:1024].rearrange("p (k n) -> p k n", k=8),
                              win_d[l][:, c0:c0 + 128].rearrange("(k p) n -> p k n", p=128))
                        P.copy(wsb[:, :, j * 128:(j + 1) * 128], sb_[:, 0:1024].rearrange("p (k n) -> p k n", k=8))
                    for (k_, c0_) in wu_pieces[pt * 8:(pt + 1) * 8]:
                        load_cast(wus[:, k_, c0_:c0_ + 704], wu_d[l][k_ * 128:(k_ + 1) * 128, c0_:c0_ + 704], stg2)
                    if pt == 0:
                        for k_ in range(8):
                            load_cast(wo[:, k_, :], wout_d[l][k_ * 128:(k_ + 1) * 128, :], stg2, fold=gf)
                    else:
                        if pt == 1:
                            P.dma(gf, modrow(l, 5))
                        for f_ in range(8 * (pt - 1), min(NF, 8 * pt)):
                            load_cast(wt2[f_ % 2], wd_d[l][f_ * 128:(f_ + 1) * 128, :], stg2, fold=gf)
                            P.dma(wd2_d[l][f_ * 128:(f_ + 1) * 128, :], wt2[f_ % 2])
                    for tc in range(NQC):
                        hc = hTc[tc % 2]
                        P.dma(hc, hT_d[:, tc * 512:(tc + 1) * 512].rearrange("(k p) t -> p k t", p=128))
                        pq, pk, pv = psb(0), psb(1), psb(2)
                        for k in range(8):
                            P.mm(pq, wsb[:, k, 0:128], hc[:, k, :], start=(k == 0), stop=(k == 7))
                        for k in range(8):
                            P.mm(pk, wsb[:, k, 128:256], hc[:, k, :], start=(k == 0), stop=(k == 7))
                        for t4 in range(4):
                            for k in range(8):
                                P.mm(pv[:, t4 * 128:(t4 + 1) * 128], hc[:, k, t4 * 128:(t4 + 1) * 128],
                                     wsb[:, k, 256:384], start=(k == 0), stop=(k == 7))
                        P.act(QT[:, tc * 512:(tc + 1) * 512], pq, AF.Copy, scale=0.125)
                        P.copy(KTz[0][0:64, tc * 512:(tc + 1) * 512], pk[0:64, :])
                        P.copy(KTz[1][64:128, tc * 512:(tc + 1) * 512], pk[64:128, :])
                        P.copy(Vp[:, tc * 4:(tc + 1) * 4, :].rearrange("p a b -> p (a b)"), pv, eng="act")
                    tiles = []
                    for hh in range(2):
                        for qc in range(NQC):
                            kbs = list(range(4 * qc + 3, -1, -1))
                            for ii, kb in enumerate(kbs):
                                tiles.append((hh, qc, kb, ii == 0, ii == len(kbs) - 1))
                    zb = [psb(3), psb(4), psb(5)]
                    ob = [psb(6), psb(7)]

                    def geom(i):
                        hh, qc, kb, first, last = tiles[i]
                        j = max(0, kb - 4 * qc)
                        return hh, qc, kb, first, last, qc * 512, j * 128

                    def A_pe(i):
                        hh, qc, kb, first, last, t0, q0 = geom(i)
                        z = zb[i % 3]
                        diag = kb >= 4 * qc
                        P.mm(z[:, q0:512], KTz[hh][:, kb * 128:(kb + 1) * 128], QT[:, t0 + q0:t0 + 512],
                             start=True, stop=not diag)
                        if diag:
                            P.mm(z[:, q0:q0 + 128], ident_bf, negstrict, start=False, stop=True)

                    def A_act(i):
                        hh, qc, kb, first, last, t0, q0 = geom(i)
                        e = ez[0]
                        P.act(e[:, q0:512], zb[i % 3][:, q0:512], AF.Exp)
                        P.act(lt[i % 3][:, q0:512], e[:, q0:512], AF.Ln, bias=1.0)

                    def B1(i):
                        hh, qc, kb, first, last, t0, q0 = geom(i)
                        z = zb[i % 3]
                        Lq = Ls[(hh * NQC + qc) % 2]
                        l_ = lt[i % 3]
                        P.mm(z[:, q0:512], ntri, l_[:, q0:512], start=False, stop=first, skip_group_check=True)
                        if not first:
                            P.mm(z[:, q0:512], nones, Lq[:, q0:512], start=False, stop=True, skip_group_check=True)
                        if not last:
                            if first:
                                if q0 > 0:
                                    P.memset(Lq[:, 0:q0], 0.0)
                                P.copy(Lq[:, q0:512], l_[:, q0:512])
                            else:
                                P.tt(Lq[:, q0:512], Lq[:, q0:512], l_[:, q0:512], ALU.add)

                    def B2(i):
                        hh, qc, kb, first, last, t0, q0 = geom(i)
                        P.act(At[i % 2][:, q0:512], zb[i % 3][:, q0:512], AF.Exp)

                    def B3(i):
                        hh, qc, kb, first, last, t0, q0 = geom(i)
                        o = ob[(hh * NQC + qc) % 2]
                        P.mm(o[:, q0:512], Vp[:, kb, :], At[i % 2][:, q0:512],
                             start=first, stop=last, skip_group_check=True)
                        if last:
                            so = osb[(hh * NQC + qc) % 2]
                            r0 = hh * 64
                            P.copy(so[r0:r0 + 64, :], o[r0:r0 + 64, :])
                            e0 = 512 + (pt * 2 + hh) * 64
                            P.dma(oT_d[e0:e0 + 64, t0:t0 + 512], so[r0:r0 + 64, :])

                    def keep_warm(k):
                        for _ in range(k):
                            P.mm(psb(0), nones, warm_rhs, start=True, stop=True)

                    n = len(tiles)
                    keep_warm(14)
                    A_pe(0)
                    A_act(0)
                    for i in range(n):
                        if i + 1 < n:
                            A_pe(i + 1)
                            A_act(i + 1)
                        B1(i)
                        B2(i)
                        if i >= 1:
                            B3(i - 1)
                        keep_warm(2)
                    B3(n - 1)
                A.release(m2)

            if 3 in phases:
                m3 = A.mark()
                a2b = A.alloc(F32, [128, D])
                sh2b = A.alloc(F32, [128, D])
                P.dma(a2b, modrow(l, 4))
                P.dma(sh2b, modrow(l, 3))
                if last_layer:
                    fgb = A.alloc(F32, [128, D])
                    P.dma(fgb, fg_d.to_broadcast([128, D]))
                wdbuf = [A.alloc(BF16, [128, D]) for _ in range(4)]
                TC = 256
                NT = TC // 128
                oTc = [A.alloc(BF16, [128, 8, TC]) for _ in range(2)]
                x0 = [A.alloc(F32, [128, D]) for _ in range(2)]
                x1 = [A.alloc(F32, [128, D]) for _ in range(NT)]
                scr = {"junk": A.alloc(BF16, [128, D]), "ss": A.alloc(F32, [128, 1]),
                       "ln": A.alloc(F32, [128, 1]), "rstd": A.alloc(F32, [128, 1]),
                       "xm": A.alloc(F32, [128, D]), "xn": A.alloc(BF16, [128, D]), "eps": epsc}
                h2T = A.alloc(BF16, [128, 8, TC])
                actT = A.alloc(BF16, [128, NF, TC])
                ea = [A.alloc(F32, [128, TC]) for _ in range(2)]
                sa = [A.alloc(F32, [128, TC]) for _ in range(2)]
                for c in range(S // TC):
                    oc = oTc[c % 2]
                    P.dma(oc, oT_d[:, c * TC:(c + 1) * TC].rearrange("(k p) t -> p k t", p=128))
                    for tbl in range(NT):
                        tb = c * NT + tbl
                        xb = x0[tbl % 2]
                        P.dma(xb, xsrc[tb * 128:(tb + 1) * 128, :])
                        for hf in range(2):
                            ps = psb(hf)
                            for e in range(8):
                                P.mm(ps, oc[:, e, tbl * 128:(tbl + 1) * 128], wo[:, e, hf * 512:(hf + 1) * 512],
                                     start=(e == 0), stop=(e == 7))
                            P.tt(x1[tbl][:, hf * 512:(hf + 1) * 512], ps, xb[:, hf * 512:(hf + 1) * 512], ALU.add)
                        if dbg and l == 0:
                            P.dma(dbg_d["d_x1"][tb * 128:(tb + 1) * 128, :], x1[tbl])
                        norm_block(x1[tbl], a2b, sh2b, scr, h2T[:, :, tbl * 128:(tbl + 1) * 128], psb(2, BF16))
                    for f in range(NF):
                        pa, pb = psb(3 + 2 * (f % 2)), psb(4 + 2 * (f % 2))
                        for k in range(8):
                            P.mm(pa[:, 0:TC], wgs[:, k, f * 128:(f + 1) * 128], h2T[:, k, :], start=(k == 0), stop=(k == 7))
                        for k in range(8):
                            P.mm(pb[:, 0:TC], wus[:, k, f * 128:(f + 1) * 128], h2T[:, k, :], start=(k == 0), stop=(k == 7))
                        e_ = ea[f % 2]
                        s_ = sa[f % 2]
                        P.act(e_, pa[:, 0:TC], AF.Exp, scale=-1.0)
                        P.act(e_, e_, AF.Ln, bias=1.0)
                        P.act(e_, e_, AF.Exp, scale=-1.0)
                        P.tt(s_, pa[:, 0:TC], e_, ALU.mult)
                        P.tt(actT[:, f, :], s_, pb[:, 0:TC], ALU.mult)
                    accb = [psb(0), psb(1), psb(2), psb(7)]
                    for f in range(NF):
                        wdb = wdbuf[f % 4]
                        P.dma(wdb, wd2_d[l][f * 128:(f + 1) * 128, :])
                        for tbl in range(NT):
                            for hf in range(2):
                                P.mm(accb[tbl * 2 + hf], actT[:, f, tbl * 128:(tbl + 1) * 128],
                                     wdb[:, hf * 512:(hf + 1) * 512], start=(f == 0), stop=(f == NF - 1))
                    for tbl in range(NT):
                        tb = c * NT + tbl
                        xo_ = x1[tbl]
                        for hf in range(2):
                            P.tt(xo_[:, hf * 512:(hf + 1) * 512], accb[tbl * 2 + hf],
                                 x1[tbl][:, hf * 512:(hf + 1) * 512], ALU.add)
                        if not last_layer:
                            P.dma(xs_d[tb * 128:(tb + 1) * 128, :], xo_)
                        else:
                            P.act(scr["junk"], xo_, AF.Square, accum_out=scr["ss"])
                            P.act(scr["ln"], scr["ss"], AF.Ln, scale=1.0 / D, bias=epsc)
                            P.act(scr["rstd"], scr["ln"], AF.Exp, scale=-0.5)
                            P.stt(scr["xm"], xo_, scr["rstd"], fgb, ALU.mult, ALU.mult)
                            P.dma(out_d[tb * 128:(tb + 1) * 128, :], scr["xm"])
                A.release(m3)
            A.release(ml)
            if dbg and l == 0:
                P.barrier()
                for k in range(8):
                    P.dma(dbg_d["d_hT"][k * 128:(k + 1) * 128, :], hT_d[k * 128:(k + 1) * 128, :])
                    P.dma(dbg_d["d_oT"][k * 128:(k + 1) * 128, :], oT_d[k * 128:(k + 1) * 128, :])
                P.barrier()
        P.barrier()
        P.emit()
    return nc, cst


_CACHE = {}


def _get(S):
    if S not in _CACHE:
        _CACHE[S] = build(S)
    return _CACHE[S]


def make_in_maps(inputs, cst, S, ncores):
    maps = []
    shared = {}
    for k in ("ada_w", "w_in", "gla_wa2", "w_out", "ffn_wg", "ffn_wu", "ffn_wd"):
        shared[k] = np.ascontiguousarray(np.asarray(inputs[k], dtype=np.float32))
    for k in ("ada_b", "norm1_g", "norm2_g", "gla_ba", "gla_norm_g", "ret_norm_g"):
        a = np.asarray(inputs[k], dtype=np.float32)
        shared[k] = np.ascontiguousarray(a.reshape(a.shape[0], 1, a.shape[1]))
    shared["final_g"] = np.ascontiguousarray(np.asarray(inputs["final_g"], dtype=np.float32).reshape(1, D))
    for k, v in cst.items():
        shared["c_" + k] = np.ascontiguousarray(v)
    x = np.asarray(inputs["x"], dtype=np.float32)
    c = np.asarray(inputs["c"], dtype=np.float32)
    for b in range(ncores):
        m = dict(shared)
        m["x"] = np.ascontiguousarray(x[b])
        m["cT"] = np.ascontiguousarray(c[b].reshape(8, 128).T)
        maps.append(m)
    return maps


def kernel(**inputs):
    x = np.asarray(inputs["x"])
    B, S, _ = x.shape
    nc, cst = _get(S)
    maps = make_in_maps(inputs, cst, S, B)
    res = run_bass_kernel_spmd(nc, maps, core_ids=list(range(B)))
    out = np.stack([np.asarray(res.results[b]["out"], dtype=np.float32) for b in range(B)], axis=0)
    return out
```

```python
import contextlib
import numpy as np
import ml_dtypes
import concourse.bass as bass
import concourse.mybir as mybir
from concourse.bass_utils import run_bass_kernel_spmd

F32 = mybir.dt.float32
BF16 = mybir.dt.bfloat16
U8 = mybir.dt.uint8
AF = mybir.ActivationFunctionType
ALU = mybir.AluOpType
AX = mybir.AxisListType
_ISZ = {F32: 4, BF16: 2, U8: 1}

D = 1024
DEPTH = 2
DFF = 2816
NF = DFF // 128
INC = 3344
EPS = 1e-6
NEG = -30000.0


class _Op:
    __slots__ = ("eng", "fn", "deps", "sig", "sigval", "dsem", "dval", "isdma", "gi")

    def __init__(self, eng, fn, isdma, gi):
        self.eng = eng
        self.fn = fn
        self.deps = set()
        self.sig = False
        self.sigval = 0
        self.dsem = -1
        self.dval = 0
        self.isdma = isdma
        self.gi = gi


class Prog:
    ENGS = ("pe", "act", "dve", "pool", "sp")
    NDMA = 40

    def __init__(self, nc, ro_names=()):
        self.nc = nc
        self.ops = []
        self.track = {}
        self.ro = set(ro_names)
        self.dma_rr = 0
        self.dma_last = [None] * self.NDMA
        self.dma_cnt = [0] * self.NDMA
        self.last_on = {e: None for e in self.ENGS}

    def _box(self, ap):
        t = ap.tensor
        ps = 1
        for s in list(t.shape)[1:]:
            ps *= int(s)
        isz = _ISZ[ap.dtype]
        off = int(ap.offset)
        p0 = off // ps
        f0 = off % ps
        pe = 0
        fe = 0
        for (st, cnt) in ap.ap:
            st = int(st)
            cnt = int(cnt)
            if cnt <= 1 or st == 0:
                continue
            if st % ps == 0:
                pe += (cnt - 1) * (st // ps)
            else:
                fe += (cnt - 1) * st
        if t.name == "PS":
            lo = (f0 * isz) // 2048 * 2048
            hi = -(-((f0 + fe + 1) * isz) // 2048) * 2048
            return (t.name, 0, 128, lo, hi)
        return (t.name, p0, p0 + pe + 1, f0 * isz, (f0 + fe + 1) * isz)

    def _access(self, op, ap, is_write):
        name, p0, p1, b0, b1 = self._box(ap)
        if name in self.ro:
            return
        d = self.track.setdefault(name, {})
        ekey = ("dma", op.gi) if op.isdma else op.eng
        dead = []
        for key, oi in d.items():
            (w, ek, q0, q1, c0, c1) = key
            if not (w or is_write):
                continue
            if q1 <= p0 or p1 <= q0 or c1 <= b0 or b1 <= c0:
                continue
            if oi == op.gi:
                continue
            if ek == ekey and op.eng == "pe":
                pass
            else:
                op.deps.add(oi)
            if is_write and q0 >= p0 and q1 <= p1 and c0 >= b0 and c1 <= b1:
                dead.append(key)
        for k in dead:
            del d[k]
        d[(is_write, ekey, p0, p1, b0, b1)] = op.gi

    def add(self, eng, fn, reads=(), writes=(), isdma=False):
        op = _Op(eng, fn, isdma, len(self.ops))
        self.ops.append(op)
        for ap in reads:
            if ap is not None and not isinstance(ap, (int, float)):
                self._access(op, ap, False)
        for ap in writes:
            self._access(op, ap, True)
        if isdma:
            k = self.dma_rr
            self.dma_rr = (self.dma_rr + 1) % self.NDMA
            if self.dma_last[k] is not None:
                op.deps.add(self.dma_last[k])
            self.dma_last[k] = op.gi
            self.dma_cnt[k] += 16
            op.dsem = k
            op.dval = self.dma_cnt[k]
            op.sig = True
        op.deps.discard(op.gi)
        for dgi in op.deps:
            self.ops[dgi].sig = True
        self.last_on[eng] = op.gi
        return op

    def barrier(self):
        lasts = [gi for gi in self.last_on.values() if gi is not None]
        dmas = [gi for gi in self.dma_last if gi is not None]
        for e in self.ENGS:
            op = _Op(e, None, False, len(self.ops))
            self.ops.append(op)
            for gi in lasts + dmas:
                o = self.ops[gi]
                if o.fn is None or (o.eng == e and not o.isdma):
                    continue
                op.deps.add(gi)
                o.sig = True

    def mm(self, out, lhsT, rhs, start=True, stop=True, **kw):
        return self.add("pe", lambda e: e.matmul(out, lhsT, rhs, start=start, stop=stop, **kw),
                        reads=[lhsT, rhs], writes=[out])

    def transpose(self, out, in_, ident):
        return self.add("pe", lambda e: e.transpose(out, in_, ident), reads=[in_, ident], writes=[out])

    def act(self, out, in_, func, bias=None, scale=None, accum_out=None):
        kw = {}
        rd = [in_]
        wr = [out]
        if bias is not None:
            kw["bias"] = bias
            rd.append(bias)
        if scale is not None:
            kw["scale"] = scale
            rd.append(scale)
        if accum_out is not None:
            kw["accum_out"] = accum_out
            wr.append(accum_out)
        return self.add("act", lambda e: e.activation(out, in_, func, **kw), reads=rd, writes=wr)

    def tt(self, out, in0, in1, op, eng="dve"):
        return self.add(eng, lambda e: e.tensor_tensor(out, in0, in1, op), reads=[in0, in1], writes=[out])

    def ts(self, out, in0, s1, s2, op0, op1=None, eng="dve"):
        kw = {}
        if op1 is not None:
            kw["op1"] = op1
        return self.add(eng, lambda e: e.tensor_scalar(out, in0, s1, s2, op0, **kw),
                        reads=[in0, s1, s2], writes=[out])

    def stt(self, out, in0, scalar, in1, op0, op1):
        return self.add("dve", lambda e: e.scalar_tensor_tensor(out, in0, scalar, in1, op0, op1),
                        reads=[in0, scalar, in1], writes=[out])

    def copy(self, out, in_, eng="dve"):
        if eng == "act":
            return self.add("act", lambda e: e.copy(out, in_), reads=[in_], writes=[out])
        return self.add(eng, lambda e: e.tensor_copy(out, in_), reads=[in_], writes=[out])

    def memset(self, ap, val, eng="dve"):
        return self.add(eng, lambda e: e.memset(ap, val), writes=[ap])

    def reduce(self, out, in_, axis, op):
        return self.add("dve", lambda e: e.tensor_reduce(out, in_, axis, op), reads=[in_], writes=[out])

    def recip(self, out, in_):
        return self.add("dve", lambda e: e.reciprocal(out, in_), reads=[in_], writes=[out])

    def dma(self, out, in_, eng="sp", **kw):
        return self.add(eng, lambda e: e.dma_start(out=out, in_=in_, **kw), reads=[in_], writes=[out],
                        isdma=True)

    def emit(self):
        nc = self.nc
        cnt = {e: 0 for e in self.ENGS}
        for op in self.ops:
            if op.isdma or op.fn is None:
                continue
            if op.sig:
                cnt[op.eng] += 1
                op.sigval = cnt[op.eng]
        with contextlib.ExitStack() as st:
            esem = {e: st.enter_context(nc.semaphore("es_" + e)) for e in self.ENGS}
            dsem = [st.enter_context(nc.semaphore("ds%d" % i)) for i in range(self.NDMA)]
            block = st.enter_context(nc.Block())
            handles = {"pe": block.tensor, "act": block.scalar, "dve": block.vector,
                       "pool": block.gpsimd, "sp": block.sync}
            ops = self.ops

            def make(ename):
                def body(eng):
                    seen = {}
                    for op in ops:
                        if op.eng != ename:
                            continue
                        need = {}
                        for dgi in op.deps:
                            d = ops[dgi]
                            if d.isdma:
                                key = ("d", d.dsem)
                                val = d.dval
                            else:
                                if d.fn is None:
                                    continue
                                key = ("e", d.eng)
                                val = d.sigval
                            if val > need.get(key, 0):
                                need[key] = val
                        for key, val in need.items():
                            if seen.get(key, 0) >= val:
                                continue
                            seen[key] = val
                            sem = dsem[key[1]] if key[0] == "d" else esem[key[1]]
                            eng.wait_ge(sem, val)
                        if op.fn is None:
                            continue
                        inst = op.fn(eng)
                        if op.isdma:
                            inst.then_inc(dsem[op.dsem], 16)
                        elif op.sig:
                            inst.then_inc(esem[op.eng], 1)
                return body

            for ename in self.ENGS:
                handles[ename](make(ename))


class Arena:
    def __init__(self, ap_u8, size):
        self.t = ap_u8
        self.size = size
        self.top = 0
        self.peak = 0

    def mark(self):
        return self.top

    def release(self, m):
        self.top = m

    def alloc(self, dtype, shape, parts=None):
        n = 1
        for s in shape[1:]:
            n *= s
        nb = n * _ISZ[dtype]
        off = (self.top + 63) // 64 * 64
        assert off + nb <= self.size, ("arena overflow", off, nb, self.size)
        self.top = off + nb
        self.peak = max(self.peak, self.top)
        v = self.t[0:shape[0], off:off + nb].bitcast(dtype)
        if len(shape) == 3:
            v = v.rearrange("p (a b) -> p a b", b=shape[2])
        elif len(shape) == 4:
            v = v.rearrange("p (a b c) -> p a b c", b=shape[2], c=shape[3])
        return v


def _consts(S):
    NB = S // 128
    c = {}
    idx = np.arange(128)
    c["ident"] = np.eye(128, dtype=np.float32)
    c["m_incl"] = (idx[:, None] <= idx[None, :]).astype(np.float32)
    c["negstrict"] = np.where(idx[:, None] >= idx[None, :], NEG, 0.0).astype(np.float32)
    c["ntri"] = -(idx[:, None] >= idx[None, :]).astype(np.float32)
    c["nones"] = -np.ones((128, 128), np.float32)
    c["trig"] = c["m_incl"] * (-1.0 / 16.0)
    c["n16"] = np.full((128, 1), -1.0 / 16.0, np.float32)
    inv = 10000.0 ** (-np.arange(0, 64, 2, dtype=np.float32) / 64.0)
    pos = np.arange(S, dtype=np.float32)
    ang = pos[:, None] * inv[None, :]
    c["cos"] = np.cos(ang).astype(np.float32).reshape(NB, 128, 32).transpose(1, 0, 2).copy()
    c["sin"] = np.sin(ang).astype(np.float32).reshape(NB, 128, 32).transpose(1, 0, 2).copy()
    gam = 1.0 - 2.0 ** (-5.0 - np.arange(4, dtype=np.float64))
    tl = np.arange(128, dtype=np.float64)
    qs = gam[None, :] ** (tl[:, None] + 1.0)
    ks = gam[None, :] ** (-(tl[:, None] + 1.0)) / 8.0
    c["rsc"] = np.concatenate([qs, ks], axis=1).astype(np.float32)
    gc = gam ** 128.0
    c["retD"] = np.stack([np.repeat(gc[0:2], 64), np.repeat(gc[2:4], 64)], axis=1).astype(np.float32)
    bm = np.zeros((128, 256), np.float32)
    for h in range(4):
        bm[32 * h:32 * h + 32, 64 * h:64 * h + 64] = 1.0
    c["bm_gla"] = bm
    bm2 = np.zeros((128, 128), np.float32)
    for h in range(2):
        bm2[64 * h:64 * h + 64, 64 * h:64 * h + 64] = 1.0
    c["bm_ret"] = bm2
    c["onerow"] = np.ones((1, 128), np.float32)
    return c


_CONST_SHAPES = None


def build(S, dbg=False, nlayers=DEPTH, phases=(0, 1, 2, 3)):
    NB = S // 128
    NQC = S // 512
    nc = bass.Bass("TRN2", target_bir_lowering=False)
    cst = _consts(S)
    ins = {}

    def din(name, shape, dt=F32):
        ins[name] = nc.dram_tensor(name, list(shape), dt, kind="ExternalInput").ap()
        return ins[name]

    x_d = din("x", [S, D])
    cT_d = din("cT", [128, 8])
    adaw_d = din("ada_w", [DEPTH, D, 6 * D])
    adab_d = din("ada_b", [DEPTH, 1, 6 * D])
    n1g_d = din("norm1_g", [DEPTH, 1, D])
    n2g_d = din("norm2_g", [DEPTH, 1, D])
    win_d = din("w_in", [DEPTH, D, INC])
    wa2_d = din("gla_wa2", [DEPTH, 16, 128])
    gba_d = din("gla_ba", [DEPTH, 1, 128])
    glag_d = din("gla_norm_g", [DEPTH, 1, 64])
    retg_d = din("ret_norm_g", [DEPTH, 1, 64])
    wout_d = din("w_out", [DEPTH, D, D])
    wg_d = din("ffn_wg", [DEPTH, D, DFF])
    wu_d = din("ffn_wu", [DEPTH, D, DFF])
    wd_d = din("ffn_wd", [DEPTH, DFF, D])
    fg_d = din("final_g", [1, D])
    cd = {k: din("c_" + k, v.shape) for k, v in cst.items()}
    out_d = nc.dram_tensor("out", [S, D], F32, kind="ExternalOutput").ap()
    xs_d = nc.dram_tensor("xs", [S, D], F32, kind="Internal").ap()
    hT_d = nc.dram_tensor("hT", [D, S], BF16, kind="Internal").ap()
    oT_d = nc.dram_tensor("oT", [D, S], BF16, kind="Internal").ap()
    mod_d = nc.dram_tensor("modrow", [DEPTH, 1, 6 * D], F32, kind="Internal").ap()
    wd2_d = nc.dram_tensor("wd2", [DEPTH, DFF, D], BF16, kind="Internal").ap()
    dbg_d = {}
    if dbg:
        dbg_d["d_hT"] = nc.dram_tensor("d_hT", [D, S], BF16, kind="ExternalOutput").ap()
        dbg_d["d_oT"] = nc.dram_tensor("d_oT", [D, S], BF16, kind="ExternalOutput").ap()
        dbg_d["d_mod"] = nc.dram_tensor("d_mod", [DEPTH, 6, D], F32, kind="ExternalOutput").ap()
        dbg_d["d_x1"] = nc.dram_tensor("d_x1", [S, D], F32, kind="ExternalOutput").ap()

    P = Prog(nc, ro_names=list(ins.keys()))
    SBYTES = 207 * 1024
    with contextlib.ExitStack() as st:
        SBt = st.enter_context(nc.sbuf_tensor("SB", [128, SBYTES], U8))
        PSt = st.enter_context(nc.psum_tensor("PS", [128, 16384], U8))
        A = Arena(SBt, SBYTES)

        def psb(b, dt=F32):
            return PSt[:, b * 2048:(b + 1) * 2048].bitcast(dt)

        ident_bf = A.alloc(BF16, [128, 128])
        ident_f = A.alloc(F32, [128, 128])
        m_incl = A.alloc(F32, [128, 128])
        negstrict = A.alloc(BF16, [128, 128])
        ntri = A.alloc(BF16, [128, 128])
        nones = A.alloc(BF16, [128, 128])
        trig = A.alloc(F32, [128, 128])
        n16 = A.alloc(F32, [128, 1])
        cos_t = A.alloc(F32, [128, NB, 32])
        sin_t = A.alloc(F32, [128, NB, 32])
        rsc = A.alloc(F32, [128, 8])
        retD = A.alloc(F32, [128, 2])
        bm_gla = A.alloc(F32, [128, 256])
        bm_ret = A.alloc(F32, [128, 128])
        onerow = A.alloc(F32, [1, 128])
        one11 = onerow[0:1, 0:1]
        mc = A.mark()
        for dst, nm in ((ident_bf, "ident"), (negstrict, "negstrict"), (ntri, "ntri"), (nones, "nones")):
            cs_ = A.alloc(F32, [128, 128])
            P.dma(cs_, cd[nm])
            P.copy(dst, cs_)
        A.release(mc)
        P.dma(ident_f, cd["ident"])
        P.dma(m_incl, cd["m_incl"])
        P.dma(trig, cd["trig"])
        P.dma(n16, cd["n16"])
        P.dma(cos_t, cd["cos"])
        P.dma(sin_t, cd["sin"])
        P.dma(rsc, cd["rsc"])
        P.dma(retD, cd["retD"])
        P.dma(bm_gla, cd["bm_gla"])
        P.dma(bm_ret, cd["bm_ret"])
        P.dma(onerow, cd["onerow"])
        epsc = A.alloc(F32, [128, 1])
        P.memset(epsc, EPS)
        warm_rhs = A.alloc(BF16, [128, 512])
        P.memset(warm_rhs, 0.0)

        m0 = A.mark()
        cT = A.alloc(F32, [128, 8])
        csT = A.alloc(F32, [128, 8])
        tmp8 = A.alloc(F32, [128, 8])
        row = A.alloc(F32, [1, 6 * D])
        grow = A.alloc(F32, [1, 2 * D + 128])
        brow = A.alloc(F32, [1, 6 * D])
        wbuf = [A.alloc(F32, [128, 8, 512]) for _ in range(2)]
        P.dma(cT, cT_d)
        P.act(tmp8, cT, AF.Exp, scale=-1.0)
        P.ts(tmp8, tmp8, 1.0, None, ALU.add)
        P.recip(tmp8, tmp8)
        P.tt(csT, cT, tmp8, ALU.mult)
        for l in range(nlayers):
            P.dma(brow, adab_d[l])
            for cg in range(12):
                wb = wbuf[cg % 2]
                P.dma(wb, adaw_d[l][:, cg * 512:(cg + 1) * 512].rearrange("(k p) n -> p k n", p=128),
                      eng="sp")
                ps = psb(cg % 2)
                for k in range(8):
                    P.mm(ps[0:1, :], csT[:, k:k + 1], wb[:, k, :], start=(k == 0), stop=(k == 7))
                P.tt(row[0:1, cg * 512:(cg + 1) * 512], ps[0:1, :], brow[0:1, cg * 512:(cg + 1) * 512], ALU.add)
            P.dma(grow[0:1, 0:D], n1g_d[l])
            P.dma(grow[0:1, D:2 * D], n2g_d[l])
            P.stt(row[0:1, D:2 * D], row[0:1, D:2 * D], 1.0, grow[0:1, 0:D], ALU.add, ALU.mult)
            P.stt(row[0:1, 4 * D:5 * D], row[0:1, 4 * D:5 * D], 1.0, grow[0:1, D:2 * D], ALU.add, ALU.mult)
            if dbg:
                P.dma(dbg_d["d_mod"][l:l + 1].rearrange("o s d -> o (s d)"), row)
            P.dma(mod_d[l], row)
        A.release(m0)

        def norm_block(xb, ab, shb, scr, hTb, psT):
            P.act(scr["junk"], xb, AF.Square, accum_out=scr["ss"])
            P.act(scr["ln"], scr["ss"], AF.Ln, scale=1.0 / D, bias=scr["eps"])
            P.act(scr["rstd"], scr["ln"], AF.Exp, scale=-0.5)
            P.stt(scr["xm"], xb, scr["rstd"], ab, ALU.mult, ALU.mult)
            P.tt(scr["xn"], scr["xm"], shb, ALU.add)
            for k in range(8):
                P.transpose(psT[:, k * 128:(k + 1) * 128], scr["xn"][:, k * 128:(k + 1) * 128], ident_bf)
            P.copy(hTb, psT.rearrange("p (a b) -> p a b", a=8), eng="act")

        def modrow(l, j):
            return mod_d[l][0:1, j * D:(j + 1) * D].to_broadcast([128, D])

        for l in range(nlayers):
            xsrc = x_d if l == 0 else xs_d
            last_layer = (l == nlayers - 1)
            ml = A.mark()
            wgs = A.alloc(BF16, [128, 8, DFF])
            cast_ctr = [0]

            def load_cast(dst, src, stg, fold=None):
                sb_ = stg[cast_ctr[0] % len(stg)]
                cast_ctr[0] += 1
                n = dst.shape[-1]
                P.dma(sb_[:, 0:n], src)
                if fold is not None:
                    P.tt(dst, sb_[:, 0:n], fold, ALU.mult)
                elif cast_ctr[0] % 2 == 0:
                    P.copy(dst, sb_[:, 0:n], eng="act")
                else:
                    P.copy(dst, sb_[:, 0:n])
            if 1 in phases:
                m1 = A.mark()
                a1b = A.alloc(F32, [128, D])
                sh1b = A.alloc(F32, [128, D])
                gnb_l = A.alloc(F32, [128, 512])
                wa2_l = A.alloc(F32, [16, 128])
                gba_l = A.alloc(F32, [1, 128])
                P.dma(a1b, modrow(l, 1))
                P.dma(sh1b, modrow(l, 0))
                for r in range(8):
                    srcg = glag_d[l] if r < 4 else retg_d[l]
                    P.dma(gnb_l[:, r * 64:(r + 1) * 64], srcg.to_broadcast([128, 64]))
                P.dma(wa2_l, wa2_d[l])
                P.dma(gba_l, gba_d[l])
                wla = A.alloc(BF16, [128, 8, 1808])
                stg1 = [A.alloc(F32, [128, 1408]) for _ in range(2)]
                for k in range(8):
                    for c0 in (0, 904):
                        load_cast(wla[:, k, c0:c0 + 904], win_d[l][k * 128:(k + 1) * 128, c0:c0 + 904], stg1)
                xbs = [A.alloc(F32, [128, D]) for _ in range(2)]
                scr = {"junk": A.alloc(BF16, [128, D]), "ss": A.alloc(F32, [128, 1]),
                       "ln": A.alloc(F32, [128, 1]), "rstd": A.alloc(F32, [128, 1]),
                       "xm": A.alloc(F32, [128, D]), "xn": A.alloc(BF16, [128, D]), "eps": epsc}
                hTbs = [A.alloc(BF16, [128, 8, 128]) for _ in range(2)]
                grT = A.alloc(F32, [16, 128])
                e1 = A.alloc(F32, [128, 128])
                lsp = A.alloc(F32, [128, 128])
                eb = A.alloc(F32, [128, 128])
                enb = A.alloc(F32, [128, 128])
                Dg = A.alloc(F32, [128, 1])
                qg = A.alloc(BF16, [128, 128])
                kg = A.alloc(BF16, [128, 128])
                KX = A.alloc(BF16, [128, 640])
                KXr = A.alloc(BF16, [128, 4, 128])
                rt = [A.alloc(F32, [128, 8, 32]) for _ in range(4)]
                qkr32 = A.alloc(F32, [128, 8, 32, 2])
                QKr = A.alloc(BF16, [128, 512])
                qkT = A.alloc(BF16, [128, 11, 128])
                scT = A.alloc(BF16, [128, 8, 128])
                Vall = A.alloc(BF16, [128, 512])
                S32 = [A.alloc(F32, [128, 256]), A.alloc(F32, [128, 128]), A.alloc(F32, [128, 128])]
                tS = [A.alloc(F32, [128, 256]), A.alloc(F32, [128, 128]), A.alloc(F32, [128, 128])]
                Sbf = [A.alloc(BF16, [128, 256]), A.alloc(BF16, [128, 128]), A.alloc(BF16, [128, 128])]
                eg = A.alloc(F32, [128, 512])
                sg = A.alloc(F32, [128, 512])
                osq = A.alloc(F32, [128, 512])
                ssum = A.alloc(F32, [128, 8])
                orstd = A.alloc(F32, [128, 8])
                on = A.alloc(F32, [128, 512])
                og = A.alloc(BF16, [128, 512])
                oTb = A.alloc(BF16, [128, 4, 128])
                P.memset(KX, 0.0)
                P.memset(KXr, 0.0)
                for i in range(3):
                    P.memset(S32[i], 0.0)
                    P.memset(Sbf[i], 0.0)
                KXd = KX.rearrange("p (h c) -> p h c", c=160)[:, :, 0:32]
                b0, b1, b2, b3, b4 = psb(0), psb(1), psb(2), psb(3), psb(4)
                b5, b6, b7 = psb(5, BF16), psb(6, BF16), psb(7, BF16)
                wg_pieces = [(k, c0) for k in range(8) for c0 in range(0, DFF, 1408)]
                def norm_in(tb_):
                    norm_block(xbs[tb_ % 2], a1b, sh1b, scr, hTbs[tb_ % 2], b5)
                    P.dma(hT_d[:, tb_ * 128:(tb_ + 1) * 128].rearrange("(k p) t -> p k t", p=128), hTbs[tb_ % 2])

                for tb in range(NB):
                    hTb = hTbs[tb % 2]
                    P.dma(xbs[tb % 2], xsrc[tb * 128:(tb + 1) * 128, :])
                    norm_in(tb)
                    npp = -(-len(wg_pieces) // NB)
                    for (k_, c0_) in wg_pieces[tb * npp:(tb + 1) * npp]:
                        load_cast(wgs[:, k_, c0_:c0_ + 1408], wg_d[l][k_ * 128:(k_ + 1) * 128, c0_:c0_ + 1408], stg1)
                    for k in range(8):
                        P.mm(b0, hTb[:, k, :], wla[:, k, 0:512], start=(k == 0), stop=(k == 7))
                    for k in range(8):
                        P.mm(b1[:, 0:256], hTb[:, k, :], wla[:, k, 512:768], start=(k == 0), stop=(k == 7))
                    for k in range(8):
                        P.mm(b1[:, 256:384], wla[:, k, 768:896], hTb[:, k, :], start=(k == 0), stop=(k == 7))
                    for k in range(8):
                        P.mm(b2, hTb[:, k, :], wla[:, k, 784:1296], start=(k == 0), stop=(k == 7))
                    for k in range(8):
                        P.mm(b3, hTb[:, k, :], wla[:, k, 1296:1808], start=(k == 0), stop=(k == 7))
                    P.copy(grT, b1[0:16, 256:384])
                    P.copy(Vall[:, 0:256], b0[:, 256:512], eng="act")
                    P.copy(Vall[:, 256:512], b3[:, 0:256], eng="act")
                    P.act(eg[:, 0:256], b1[:, 0:256], AF.Exp, scale=-1.0)
                    P.act(eg[:, 256:512], b3[:, 256:512], AF.Exp, scale=-1.0)
                    P.act(eg, eg, AF.Ln, bias=1.0)
                    P.act(eg, eg, AF.Exp, scale=-1.0)
                    P.tt(sg[:, 0:256], b1[:, 0:256], eg[:, 0:256], ALU.mult)
                    P.tt(sg[:, 256:512], b3[:, 256:512], eg[:, 256:512], ALU.mult)
                    P.mm(b4[:, 0:128], grT, wa2_l, start=True, stop=False)
                    P.mm(b4[:, 0:128], onerow[0:1, :], gba_l, start=False, stop=True)
                    P.act(e1, b4[:, 0:128], AF.Exp, scale=-1.0)
                    P.act(lsp, e1, AF.Ln, bias=1.0)
                    P.mm(b4[:, 128:256], trig, lsp, start=True, stop=True)
                    P.mm(b4[:, 256:257], lsp, n16, start=True, stop=True)
                    P.act(eb, b4[:, 128:256], AF.Exp)
                    P.act(enb, b4[:, 128:256], AF.Exp, scale=-1.0)
                    P.act(Dg, b4[:, 256:257], AF.Exp)
                    P.stt(qg, b0[:, 0:128], 32.0 ** -0.5, eb, ALU.mult, ALU.mult)
                    P.tt(kg, b0[:, 128:256], enb, ALU.mult)
                    P.copy(KXd, kg.rearrange("p (h d) -> p h d", h=4), eng="act")
                    qk4 = b2.rearrange("p (h i two) -> p h i two", h=8, two=2)
                    ev, od = qk4[:, :, :, 0], qk4[:, :, :, 1]
                    cb = cos_t[:, tb, :].unsqueeze(1).broadcast_to([128, 8, 32])
                    sb_ = sin_t[:, tb, :].unsqueeze(1).broadcast_to([128, 8, 32])
                    P.tt(rt[0], ev, cb, ALU.mult)
                    P.tt(rt[1], od, sb_, ALU.mult)
                    P.tt(rt[2], ev, sb_, ALU.mult)
                    P.tt(rt[3], od, cb, ALU.mult)
                    P.tt(qkr32[:, :, :, 0], rt[0], rt[1], ALU.subtract)
                    P.tt(qkr32[:, :, :, 1], rt[2], rt[3], ALU.add)
                    P.tt(QKr.rearrange("p (h d) -> p h d", h=8), qkr32.rearrange("p h i two -> p h (i two)"),
                         rsc.unsqueeze(2).broadcast_to([128, 8, 64]), ALU.mult)
                    P.transpose(b6[:, 0:128], qg, ident_bf)
                    for h in range(4):
                        P.transpose(b6[:, (1 + h) * 128:(2 + h) * 128], KX[:, h * 128:(h + 1) * 128], ident_bf)
                    for h in range(4):
                        c0 = (h % 2) * 64
                        P.copy(KXr[:, h, c0:c0 + 64], QKr[:, 256 + h * 64:320 + h * 64], eng=("act" if h % 2 else "dve"))
                    for j in range(2):
                        P.transpose(b6[:, (5 + j) * 128:(6 + j) * 128], QKr[:, j * 128:(j + 1) * 128], ident_bf)
                    P.transpose(b6[:, 7 * 128:8 * 128], KXr[:, 0, :], ident_bf)
                    for h in range(1, 4):
                        P.transpose(b7[:, (h - 1) * 128:h * 128], KXr[:, h, :], ident_bf)
                    P.copy(qkT[:, 0:8, :], b6.rearrange("p (a b) -> p a b", a=8), eng="act")
                    P.copy(qkT[:, 8:11, :], b7[:, 0:384].rearrange("p (a b) -> p a b", a=3), eng="act")
                    for h in range(4):
                        P.mm(b0[:, h * 128:(h + 1) * 128], qkT[:, 1 + h, :], qkT[:, 0, :], start=True, stop=True)
                    for h in range(4):
                        P.mm(b2[:, h * 128:(h + 1) * 128], qkT[:, 7 + h, :], qkT[:, 5 + h // 2, :],
                             start=True, stop=True)
                    mb = m_incl.unsqueeze(1).broadcast_to([128, 4, 128])
                    P.tt(scT[:, 0:4, :], b0.rearrange("p (h t) -> p h t", h=4), mb, ALU.mult)
                    P.tt(scT[:, 4:8, :], b2.rearrange("p (h t) -> p h t", h=4), mb, ALU.mult)
                    for h in range(8):
                        if h < 4:
                            ql, sl = qkT[:, 0, :], Sbf[0][:, h * 64:(h + 1) * 64]
                        else:
                            g = (h - 4) // 2
                            ql, sl = qkT[:, 5 + g, :], Sbf[1 + g][:, ((h - 4) % 2) * 64:((h - 4) % 2 + 1) * 64]
                        P.mm(b1[:, h * 64:(h + 1) * 64], scT[:, h, :], Vall[:, h * 64:(h + 1) * 64],
                             start=True, stop=False)
                        P.mm(b1[:, h * 64:(h + 1) * 64], ql, sl, start=False, stop=True)
                    P.mm(b3[:, 0:256], kg, Vall[:, 0:256], start=True, stop=True)
                    P.mm(b3[:, 256:384], QKr[:, 256:384], Vall[:, 256:384], start=True, stop=True)
                    P.mm(b3[:, 384:512], QKr[:, 384:512], Vall[:, 384:512], start=True, stop=True)
                    P.tt(tS[0], b3[:, 0:256], S32[0], ALU.add)
                    P.stt(S32[0], tS[0], Dg, bm_gla, ALU.mult, ALU.mult)
                    P.copy(Sbf[0], S32[0], eng="act")
                    for g in range(2):
                        P.tt(tS[1 + g], b3[:, 256 + g * 128:384 + g * 128], S32[1 + g], ALU.add)
                        P.stt(S32[1 + g], tS[1 + g], retD[:, g:g + 1], bm_ret, ALU.mult, ALU.mult)
                        P.copy(Sbf[1 + g], S32[1 + g], eng="act")
                    P.act(osq, b1, AF.Square)
                    P.reduce(ssum, osq.rearrange("p (h v) -> p h v", h=8), AX.X, ALU.add)
                    P.act(orstd, ssum, AF.Ln, scale=1.0 / 64.0, bias=epsc)
                    P.act(orstd, orstd, AF.Exp, scale=-0.5)
                    P.tt(on.rearrange("p (h v) -> p h v", h=8), b1.rearrange("p (h v) -> p h v", h=8),
                         orstd.unsqueeze(2).broadcast_to([128, 8, 64]), ALU.mult)
                    P.tt(on, on, gnb_l, ALU.mult)
                    P.tt(og, on, sg, ALU.mult)
                    for j in range(4):
                        P.transpose(b7[:, (3 + j) * 128:(4 + j) * 128], og[:, j * 128:(j + 1) * 128], ident_bf)
                    P.copy(oTb, b7[:, 384:896].rearrange("p (a b) -> p a b", a=4), eng="act")
                    P.dma(oT_d[0:512, tb * 128:(tb + 1) * 128].rearrange("(k p) t -> p k t", p=128), oTb)
                A.release(m1)

            wus = A.alloc(BF16, [128, 8, DFF])
            wo = A.alloc(BF16, [128, 8, D])
            if 2 in phases:
                m2 = A.mark()
                wsb = A.alloc(BF16, [128, 8, 384])
                hTc = [A.alloc(BF16, [128, 8, 512]) for _ in range(2)]
                QT = A.alloc(BF16, [128, S])
                KTz = [A.alloc(BF16, [128, S]) for _ in range(2)]
                P.memset(KTz[0][64:128, :], 0.0)
                P.memset(KTz[1][0:64, :], 0.0)
                Vp = A.alloc(BF16, [128, NB, 128])
                ez = [A.alloc(F32, [128, 512]) for _ in range(1)]
                lt = [A.alloc(BF16, [128, 512]) for _ in range(3)]
                Ls = [A.alloc(BF16, [128, 512]) for _ in range(2)]
                At = [A.alloc(BF16, [128, 512]) for _ in range(2)]
                osb = [A.alloc(BF16, [128, 512]) for _ in range(2)]
                stg2 = [A.alloc(F32, [128, 1024]) for _ in range(2)]
                gf = A.alloc(F32, [128, D])
                wt2 = [A.alloc(BF16, [128, D]) for _ in range(2)]
                P.dma(gf, modrow(l, 2))
                wu_pieces = [(k, c0) for k in range(8) for c0 in range(0, DFF, 704)]
                for pt in range(4):
                    for j in range(3):
                        c0 = 1808 + j * 512 + pt * 128
                        sb_ = stg2[cast_ctr[0] % 2]
                        cast_ctr[0] += 1
                        P.dma(sb_[:, 0:1024].rearrange("p (k n) -> p k n", k=8),
                              win_d[l][:, c0:c0 + 128].rearrange("(k p) n -> p k n", p=128))
                        P.copy(wsb[:, :, j * 128:(j + 1) * 128], sb_[:, 0:1024].rearrange("p (k n) -> p k n", k=8))
                    for (k_, c0_) in wu_pieces[pt * 8:(pt + 1) * 8]:
                        load_cast(wus[:, k_, c0_:c0_ + 704], wu_d[l][k_ * 128:(k_ + 1) * 128, c0_:c0_ + 704], stg2)
                    if pt == 0:
                        for k_ in range(8):
                            load_cast(wo[:, k_, :], wout_d[l][k_ * 128:(k_ + 1) * 128, :], stg2, fold=gf)
                    else:
                        if pt == 1:
                            P.dma(gf, modrow(l, 5))
                        for f_ in range(8 * (pt - 1), min(NF, 8 * pt)):
                            load_cast(wt2[f_ % 2], wd_d[l][f_ * 128:(f_ + 1) * 128, :], stg2, fold=gf)
                            P.dma(wd2_d[l][f_ * 128:(f_ + 1) * 128, :], wt2[f_ % 2])
                    for tc in range(NQC):
                        hc = hTc[tc % 2]
                        P.dma(hc, hT_d[:, tc * 512:(tc + 1) * 512].rearrange("(k p) t -> p k t", p=128))
                        pq, pk, pv = psb(0), psb(1), psb(2)
                        for k in range(8):
                            P.mm(pq, wsb[:, k, 0:128], hc[:, k, :], start=(k == 0), stop=(k == 7))
                        for k in range(8):
                            P.mm(pk, wsb[:, k, 128:256], hc[:, k, :], start=(k == 0), stop=(k == 7))
                        for t4 in range(4):
                            for k in range(8):
                                P.mm(pv[:, t4 * 128:(t4 + 1) * 128], hc[:, k, t4 * 128:(t4 + 1) * 128],
                                     wsb[:, k, 256:384], start=(k == 0), stop=(k == 7))
                        P.ts(QT[:, tc * 512:(tc + 1) * 512], pq, 0.125, None, ALU.mult)
                        P.copy(KTz[0][0:64, tc * 512:(tc + 1) * 512], pk[0:64, :])
                        P.copy(KTz[1][64:128, tc * 512:(tc + 1) * 512], pk[64:128, :])
                        P.copy(Vp[:, tc * 4:(tc + 1) * 4, :].rearrange("p a b -> p (a b)"), pv)
                    tiles = []
                    for hh in range(2):
                        for qc in range(NQC):
                            kbs = list(range(4 * qc + 3, -1, -1))
                            for ii, kb in enumerate(kbs):
                                tiles.append((hh, qc, kb, ii == 0, ii == len(kbs) - 1))
                    zb = [psb(3), psb(4), psb(5)]
                    ob = [psb(6), psb(7)]

                    def geom(i):
                        hh, qc, kb, first, last = tiles[i]
                        j = max(0, kb - 4 * qc)
                        return hh, qc, kb, first, last, qc * 512, j * 128

                    def A_pe(i):
                        hh, qc, kb, first, last, t0, q0 = geom(i)
                        z = zb[i % 3]
                        diag = kb >= 4 * qc
                        P.mm(z[:, q0:512], KTz[hh][:, kb * 128:(kb + 1) * 128], QT[:, t0 + q0:t0 + 512],
                             start=True, stop=not diag)
                        if diag:
                            P.mm(z[:, q0:q0 + 128], ident_bf, negstrict, start=False, stop=True)

                    def A_act(i):
                        hh, qc, kb, first, last, t0, q0 = geom(i)
                        e = ez[0]
                        P.act(e[:, q0:512], zb[i % 3][:, q0:512], AF.Exp)
                        P.act(lt[i % 3][:, q0:512], e[:, q0:512], AF.Ln, bias=1.0)

                    def B1(i):
                        hh, qc, kb, first, last, t0, q0 = geom(i)
                        z = zb[i % 3]
                        Lq = Ls[(hh * NQC + qc) % 2]
                        l_ = lt[i % 3]
                        P.mm(z[:, q0:512], ntri, l_[:, q0:512], start=False, stop=first, skip_group_check=True)
                        if not first:
                            P.mm(z[:, q0:512], nones, Lq[:, q0:512], start=False, stop=True, skip_group_check=True)
                        if not last:
                            if first:
                                if q0 > 0:
                                    P.memset(Lq[:, 0:q0], 0.0)
                                P.copy(Lq[:, q0:512], l_[:, q0:512])
                            else:
                                P.tt(Lq[:, q0:512], Lq[:, q0:512], l_[:, q0:512], ALU.add)

                    def B2(i):
                        hh, qc, kb, first, last, t0, q0 = geom(i)
                        P.act(At[i % 2][:, q0:512], zb[i % 3][:, q0:512], AF.Exp)

                    def B3(i):
                        hh, qc, kb, first, last, t0, q0 = geom(i)
                        o = ob[(hh * NQC + qc) % 2]
                        P.mm(o[:, q0:512], Vp[:, kb, :], At[i % 2][:, q0:512],
                             start=first, stop=last, skip_group_check=True)
                        if last:
                            so = osb[(hh * NQC + qc) % 2]
                            r0 = hh * 64
                            P.copy(so[r0:r0 + 64, :], o[r0:r0 + 64, :])
                            e0 = 512 + (pt * 2 + hh) * 64
                            P.dma(oT_d[e0:e0 + 64, t0:t0 + 512], so[r0:r0 + 64, :])

                    def keep_warm(k):
                        for _ in range(k):
                            P.mm(psb(0), nones, warm_rhs, start=True, stop=True)

                    n = len(tiles)
                    keep_warm(14)
                    A_pe(0)
                    A_act(0)
                    for i in range(n):
                        if i + 1 < n:
                            A_pe(i + 1)
                            A_act(i + 1)
                        B1(i)
                        B2(i)
                        if i >= 1:
                            B3(i - 1)
                        keep_warm(2)
                    B3(n - 1)
                A.release(m2)

            if 3 in phases:
                m3 = A.mark()
                a2b = A.alloc(F32, [128, D])
                sh2b = A.alloc(F32, [128, D])
                P.dma(a2b, modrow(l, 4))
                P.dma(sh2b, modrow(l, 3))
                if last_layer:
                    fgb = A.alloc(F32, [128, D])
                    P.dma(fgb, fg_d.to_broadcast([128, D]))
                wdbuf = [A.alloc(BF16, [128, D]) for _ in range(4)]
                TC = 256
                NT = TC // 128
                oTc = [A.alloc(BF16, [128, 8, TC]) for _ in range(2)]
                x0 = [A.alloc(F32, [128, D]) for _ in range(2)]
                x1 = [A.alloc(F32, [128, D]) for _ in range(NT)]
                scr = {"junk": A.alloc(BF16, [128, D]), "ss": A.alloc(F32, [128, 1]),
                       "ln": A.alloc(F32, [128, 1]), "rstd": A.alloc(F32, [128, 1]),
                       "xm": A.alloc(F32, [128, D]), "xn": A.alloc(BF16, [128, D]), "eps": epsc}
                h2T = A.alloc(BF16, [128, 8, TC])
                actT = A.alloc(BF16, [128, NF, TC])
                ea = [A.alloc(F32, [128, TC]) for _ in range(2)]
                sa = [A.alloc(F32, [128, TC]) for _ in range(2)]
                for c in range(S // TC):
                    oc = oTc[c % 2]
                    P.dma(oc, oT_d[:, c * TC:(c + 1) * TC].rearrange("(k p) t -> p k t", p=128))
                    for tbl in range(NT):
                        tb = c * NT + tbl
                        xb = x0[tbl % 2]
                        P.dma(xb, xsrc[tb * 128:(tb + 1) * 128, :])
                        for hf in range(2):
                            ps = psb(hf)
                            for e in range(8):
                                P.mm(ps, oc[:, e, tbl * 128:(tbl + 1) * 128], wo[:, e, hf * 512:(hf + 1) * 512],
                                     start=(e == 0), stop=(e == 7))
                            P.tt(x1[tbl][:, hf * 512:(hf + 1) * 512], ps, xb[:, hf * 512:(hf + 1) * 512], ALU.add)
                        if dbg and l == 0:
                            P.dma(dbg_d["d_x1"][tb * 128:(tb + 1) * 128, :], x1[tbl])
                        norm_block(x1[tbl], a2b, sh2b, scr, h2T[:, :, tbl * 128:(tbl + 1) * 128], psb(2, BF16))
                    for f in range(NF):
                        pa, pb = psb(3 + 2 * (f % 2)), psb(4 + 2 * (f % 2))
                        for k in range(8):
                            P.mm(pa[:, 0:TC], wgs[:, k, f * 128:(f + 1) * 128], h2T[:, k, :], start=(k == 0), stop=(k == 7))
                        for k in range(8):
                            P.mm(pb[:, 0:TC], wus[:, k, f * 128:(f + 1) * 128], h2T[:, k, :], start=(k == 0), stop=(k == 7))
                        e_ = ea[f % 2]
                        s_ = sa[f % 2]
                        P.act(e_, pa[:, 0:TC], AF.Exp, scale=-1.0)
                        P.act(e_, e_, AF.Ln, bias=1.0)
                        P.act(e_, e_, AF.Exp, scale=-1.0)
                        P.tt(s_, pa[:, 0:TC], e_, ALU.mult)
                        P.tt(actT[:, f, :], s_, pb[:, 0:TC], ALU.mult)
                    accb = [psb(0), psb(1), psb(2), psb(7)]
                    for f in range(NF):
                        wdb = wdbuf[f % 4]
                        P.dma(wdb, wd2_d[l][f * 128:(f + 1) * 128, :])
                        for tbl in range(NT):
                            for hf in range(2):
                                P.mm(accb[tbl * 2 + hf], actT[:, f, tbl * 128:(tbl + 1) * 128],
                                     wdb[:, hf * 512:(hf + 1) * 512], start=(f == 0), stop=(f == NF - 1))
                    for tbl in range(NT):
                        tb = c * NT + tbl
                        xo_ = x1[tbl]
                        for hf in range(2):
                            P.tt(xo_[:, hf * 512:(hf + 1) * 512], accb[tbl * 2 + hf],
                                 x1[tbl][:, hf * 512:(hf + 1) * 512], ALU.add)
                        if not last_layer:
                            P.dma(xs_d[tb * 128:(tb + 1) * 128, :], xo_)
                        else:
                            P.act(scr["junk"], xo_, AF.Square, accum_out=scr["ss"])
                            P.act(scr["ln"], scr["ss"], AF.Ln, scale=1.0 / D, bias=epsc)
                            P.act(scr["rstd"], scr["ln"], AF.Exp, scale=-0.5)
                            P.stt(scr["xm"], xo_, scr["rstd"], fgb, ALU.mult, ALU.mult)
                            P.dma(out_d[tb * 128:(tb + 1) * 128, :], scr["xm"])
                A.release(m3)
            A.release(ml)
            if dbg and l == 0:
                P.barrier()
                for k in range(8):
                    P.dma(dbg_d["d_hT"][k * 128:(k + 1) * 128, :], hT_d[k * 128:(k + 1) * 128, :])
                    P.dma(dbg_d["d_oT"][k * 128:(k + 1) * 128, :], oT_d[k * 128:(k + 1) * 128, :])
                P.barrier()
        P.barrier()
        P.emit()
    return nc, cst


_CACHE = {}


def _get(S):
    if S not in _CACHE:
        _CACHE[S] = build(S)
    return _CACHE[S]


def make_in_maps(inputs, cst, S, ncores):
    maps = []
    shared = {}
    for k in ("ada_w", "w_in", "gla_wa2", "w_out", "ffn_wg", "ffn_wu", "ffn_wd"):
        shared[k] = np.ascontiguousarray(np.asarray(inputs[k], dtype=np.float32))
    for k in ("ada_b", "norm1_g", "norm2_g", "gla_ba", "gla_norm_g", "ret_norm_g"):
        a = np.asarray(inputs[k], dtype=np.float32)
        shared[k] = np.ascontiguousarray(a.reshape(a.shape[0], 1, a.shape[1]))
    shared["final_g"] = np.ascontiguousarray(np.asarray(inputs["final_g"], dtype=np.float32).reshape(1, D))
    for k, v in cst.items():
        shared["c_" + k] = np.ascontiguousarray(v)
    x = np.asarray(inputs["x"], dtype=np.float32)
    c = np.asarray(inputs["c"], dtype=np.float32)
    for b in range(ncores):
        m = dict(shared)
        m["x"] = np.ascontiguousarray(x[b])
        m["cT"] = np.ascontiguousarray(c[b].reshape(8, 128).T)
        maps.append(m)
    return maps


def kernel(**inputs):
    x = np.asarray(inputs["x"])
    B, S, _ = x.shape
    nc, cst = _get(S)
    maps = make_in_maps(inputs, cst, S, B)
    res = run_bass_kernel_spmd(nc, maps, core_ids=list(range(B)))
    out = np.stack([np.asarray(res.results[b]["out"], dtype=np.float32) for b in range(B)], axis=0)
    return out
```
